# Optimizing a Trainium2 kernel written in Bass

```python
import jax, jax.numpy as jnp
from jax import lax
import numpy as np

D_MODEL = 1024
BATCH = 4
SEQ = 4096
DEPTH = 2

CHUNK = 64
Q_BLOCK = 128
EPS = 1e-6

A_HEADS = 8
A_NOPE = 64
A_ROPE = 32
A_VDIM = 64
A_QLORA = 256
A_KVLORA = 128
ROPE_THETA = 10000.0

B_HEADS = 4
B_DIM = 64

POOL_WINDOWS = (2, 4, 8, 16)
POOL_CH = 64

A_WIDTH = A_HEADS * A_VDIM
B_WIDTH = B_HEADS * B_DIM
C_WIDTH = len(POOL_WINDOWS) * POOL_CH
D_MIX = A_WIDTH + B_WIDTH + C_WIDTH

A_IN = A_QLORA + A_KVLORA + A_ROPE
B_IN = 3 * B_WIDTH
D_IN = A_IN + B_IN + C_WIDTH

N_GROUPS = 4
EXPERTS_PER_GROUP = 8
TOP_K_INNER = 2
D_EXPERT = 256

kernel_name = 'hybrid_mla_stickbreak_pool_hmoe'


def rms_norm(x, g):
    xf = x.astype(jnp.float32)
    y = xf * lax.rsqrt(jnp.mean(xf * xf, axis=-1, keepdims=True) + EPS)
    return (y * g.astype(jnp.float32)).astype(x.dtype)


def rope_tables(seq):
    pos = jnp.arange(seq, dtype=jnp.float32)
    inv = ROPE_THETA ** (-jnp.arange(0, A_ROPE, 2, dtype=jnp.float32) / A_ROPE)
    ang = pos[:, None] * inv[None, :]
    return jnp.cos(ang), jnp.sin(ang)


def apply_rope(x, cos, sin):
    half = x.shape[-1] // 2
    x1 = x[..., :half].astype(jnp.float32)
    x2 = x[..., half:].astype(jnp.float32)
    out = jnp.concatenate([x1 * cos - x2 * sin, x2 * cos + x1 * sin], axis=-1)
    return out.astype(x.dtype)


def mla_mixer(c_q, c_kv, k_r, q_norm_g, w_uq, kv_norm_g, w_ukv):
    B, S, _ = c_q.shape
    cos, sin = rope_tables(S)
    q = jnp.einsum('bsr,rf->bsf', rms_norm(c_q, q_norm_g), w_uq).reshape(B, S, A_HEADS, A_NOPE + A_ROPE)
    q = jnp.concatenate([q[..., :A_NOPE], apply_rope(q[..., A_NOPE:], cos[None, :, None], sin[None, :, None])], axis=-1)
    kv = jnp.einsum('bsr,rf->bsf', rms_norm(c_kv, kv_norm_g), w_ukv).reshape(B, S, A_HEADS, A_NOPE + A_VDIM)
    k_rope = apply_rope(k_r, cos[None], sin[None])
    k = jnp.concatenate([kv[..., :A_NOPE], jnp.broadcast_to(k_rope[:, :, None, :], (B, S, A_HEADS, A_ROPE))], axis=-1)
    v = kv[..., A_NOPE:]
    scale = (A_NOPE + A_ROPE) ** -0.5
    chunk_id = jnp.arange(S) // CHUNK
    outs = []
    for i in range(S // Q_BLOCK):
        q0, q1 = i * Q_BLOCK, (i + 1) * Q_BLOCK
        s = jnp.einsum('bqhd,bkhd->bhqk', q[:, q0:q1], k[:, :q1]).astype(jnp.float32) * scale
        allowed = chunk_id[:q1][None, :] <= chunk_id[q0:q1][:, None]
        s = jnp.where(allowed, s, -1e30)
        p = jax.nn.softmax(s, axis=-1).astype(v.dtype)
        outs.append(jnp.einsum('bhqk,bkhd->bqhd', p, v[:, :q1]))
    return jnp.concatenate(outs, axis=1).reshape(B, S, A_WIDTH)


def stick_breaking_mixer(q, k, v):
    B, S = q.shape[0], q.shape[1]
    pos = jnp.arange(S)
    scale = B_DIM ** -0.5
    outs = []
    for i in range(S // Q_BLOCK):
        q0, q1 = i * Q_BLOCK, (i + 1) * Q_BLOCK
        z = jnp.einsum('bqhd,bkhd->bhqk', q[:, q0:q1], k[:, :q1]).astype(jnp.float32) * scale
        strict = pos[:q1][None, :] < pos[q0:q1][:, None]
        log_1m_beta = jnp.where(strict, -jax.nn.softplus(z), 0.0)
        between = lax.cumsum(log_1m_beta, axis=3, reverse=True) - log_1m_beta
        a = jnp.where(strict, jnp.exp(jax.nn.log_sigmoid(z) + between), 0.0).astype(v.dtype)
        outs.append(jnp.einsum('bhqk,bkhd->bqhd', a, v[:, :q1]))
    return jnp.concatenate(outs, axis=1).reshape(B, S, B_WIDTH)


def pool_mixer(u, pool_w, pool_scale):
    B, S, _ = u.shape
    uf = u.astype(jnp.float32)
    cs = jnp.concatenate([jnp.zeros((B, 1, C_WIDTH), jnp.float32), jnp.cumsum(uf, axis=1)], axis=1)
    pos = jnp.arange(S, dtype=jnp.float32)
    groups = []
    for g, w in enumerate(POOL_WINDOWS):
        c = cs[..., g * POOL_CH:(g + 1) * POOL_CH]
        lag = jnp.pad(c[:, :S - w + 1], ((0, 0), (w - 1, 0), (0, 0)))
        count = jnp.minimum(pos + 1.0, float(w))[None, :, None]
        mean = (c[:, 1:] - lag) / count
        groups.append(mean - uf[..., g * POOL_CH:(g + 1) * POOL_CH])
    d = jnp.stack(groups, axis=2).astype(u.dtype)
    y = jnp.einsum('bsgc,gcd->bsgd', d, pool_w).reshape(B, S, C_WIDTH)
    return y * pool_scale


def hier_moe(h, w_group, b_group, w_expert, b_expert, w_gate, w_up, w_down):
    B, S, D = h.shape
    t = h.reshape(B * S, D)
    g_prob = jax.nn.softmax((t @ w_group + b_group).astype(jnp.float32), axis=-1)
    p_sel, g_idx = lax.top_k(g_prob, 1)
    p_sel, g_idx = p_sel[:, 0], g_idx[:, 0]
    e_logits = (t @ w_expert + b_expert).astype(jnp.float32).reshape(B * S, N_GROUPS, EXPERTS_PER_GROUP)
    e_in = jnp.take_along_axis(e_logits, g_idx[:, None, None], axis=1)[:, 0]
    e_prob = jax.nn.softmax(e_in, axis=-1)
    w_k, i_k = lax.top_k(e_prob, TOP_K_INNER)
    w_k = w_k / jnp.sum(w_k, axis=-1, keepdims=True)
    inner = jnp.sum(jax.nn.one_hot(i_k, EXPERTS_PER_GROUP, dtype=jnp.float32) * w_k[..., None], axis=1)
    combine = (p_sel[:, None, None] * jax.nn.one_hot(g_idx, N_GROUPS, dtype=jnp.float32)[:, :, None]
               * inner[:, None, :]).astype(t.dtype)
    y = jnp.zeros_like(t)
    for g in range(N_GROUPS):
        a = jnp.einsum('nd,edf->nef', t, w_gate[g])
        b = jnp.einsum('nd,edf->nef', t, w_up[g])
        hid = jax.nn.silu(a) * b * combine[:, g, :, None]
        y = y + jnp.einsum('nef,efd->nd', hid, w_down[g])
    return y.reshape(B, S, D)


def hybrid_layer(x, norm1_g, w_in, q_norm_g, w_uq, kv_norm_g, w_ukv, pool_w, pool_scale, w_out,
                 norm2_g, w_group, b_group, w_expert, b_expert, w_gate, w_up, w_down):
    B, S, _ = x.shape
    hn = rms_norm(x, norm1_g)
    proj = jnp.einsum('bsd,df->bsf', hn, w_in)
    o1 = A_QLORA
    o2 = o1 + A_KVLORA
    o3 = o2 + A_ROPE
    o4 = o3 + B_IN
    ya = mla_mixer(proj[..., :o1], proj[..., o1:o2], proj[..., o2:o3], q_norm_g, w_uq, kv_norm_g, w_ukv)
    qkv_b = proj[..., o3:o4].reshape(B, S, 3, B_HEADS, B_DIM)
    yb = stick_breaking_mixer(qkv_b[:, :, 0], qkv_b[:, :, 1], qkv_b[:, :, 2])
    yc = pool_mixer(proj[..., o4:], pool_w, pool_scale)
    mixed = jnp.concatenate([ya, yb, yc], axis=-1)
    x = x + jnp.einsum('bsm,md->bsd', mixed, w_out)
    x = x + hier_moe(rms_norm(x, norm2_g), w_group, b_group, w_expert, b_expert, w_gate, w_up, w_down)
    return x


def setup_inputs(seed: int = 0) -> dict:
    key = jax.random.key(seed)
    ks = jax.random.split(key, 20)
    L, D = DEPTH, D_MODEL

    def nrm(k, shape, fan_in):
        return jax.random.normal(k, shape, jnp.float32) * (fan_in ** -0.5)

    def gain(k, shape):
        return 1.0 + 0.02 * jax.random.normal(k, shape, jnp.float32)

    return {
        'x': jax.random.normal(ks[0], (BATCH, SEQ, D), jnp.float32),
        'norm1_g': gain(ks[1], (L, D)),
        'w_in': nrm(ks[2], (L, D, D_IN), D),
        'q_norm_g': gain(ks[3], (L, A_QLORA)),
        'w_uq': nrm(ks[4], (L, A_QLORA, A_HEADS * (A_NOPE + A_ROPE)), A_QLORA),
        'kv_norm_g': gain(ks[5], (L, A_KVLORA)),
        'w_ukv': nrm(ks[6], (L, A_KVLORA, A_HEADS * (A_NOPE + A_VDIM)), A_KVLORA),
        'pool_w': nrm(ks[7], (L, len(POOL_WINDOWS), POOL_CH, POOL_CH), POOL_CH),
        'pool_scale': gain(ks[8], (L, C_WIDTH)),
        'w_out': nrm(ks[9], (L, D_MIX, D), D_MIX),
        'norm2_g': gain(ks[10], (L, D)),
        'w_group': nrm(ks[11], (L, D, N_GROUPS), D),
        'b_group': 0.01 * jax.random.normal(ks[12], (L, N_GROUPS), jnp.float32),
        'w_expert': nrm(ks[13], (L, D, N_GROUPS * EXPERTS_PER_GROUP), D),
        'b_expert': 0.01 * jax.random.normal(ks[14], (L, N_GROUPS * EXPERTS_PER_GROUP), jnp.float32),
        'w_gate': nrm(ks[15], (L, N_GROUPS, EXPERTS_PER_GROUP, D, D_EXPERT), D),
        'w_up': nrm(ks[16], (L, N_GROUPS, EXPERTS_PER_GROUP, D, D_EXPERT), D),
        'w_down': nrm(ks[17], (L, N_GROUPS, EXPERTS_PER_GROUP, D_EXPERT, D), D_EXPERT),
        'final_g': gain(ks[18], (D,)),
    }


def reference(x, norm1_g, w_in, q_norm_g, w_uq, kv_norm_g, w_ukv, pool_w, pool_scale, w_out,
              norm2_g, w_group, b_group, w_expert, b_expert, w_gate, w_up, w_down, final_g):
    for l in range(DEPTH):
        x = hybrid_layer(x, norm1_g[l], w_in[l], q_norm_g[l], w_uq[l], kv_norm_g[l], w_ukv[l],
                         pool_w[l], pool_scale[l], w_out[l], norm2_g[l], w_group[l], b_group[l],
                         w_expert[l], b_expert[l], w_gate[l], w_up[l], w_down[l])
    return rms_norm(x, final_g)
```

```python
import numpy as np
import ml_dtypes
import concourse.bass as bass
import concourse.mybir as mybir
from concourse.bass_utils import run_bass_kernel_spmd

F32 = mybir.dt.float32
BF16 = mybir.dt.bfloat16
AF = mybir.ActivationFunctionType
ALU = mybir.AluOpType

D = 1024
SEQ = 4096
NBLK = 32
NOWN = 16
EPS = 1e-6
A_HEADS = 8
B_HEADS = 4
NEXP = 32
SB_BASE = 16512
SB_TOP = 229344


def g_of(r, i):
    return 2 * i + (i % 2) if r == 0 else 2 * i + 1 - (i % 2)


class Ctx:
    def __init__(self, nc):
        self.nc = nc
        self.eng = {'pe': nc.tensor, 'act': nc.scalar, 'dve': nc.vector, 'pool': nc.gpsimd, 'sp': nc.sync}
        self.semh = {}
        self.cnt = {}
        for e in self.eng:
            self.semh['s_' + e] = nc.alloc_semaphore('s_' + e)
            self.cnt['s_' + e] = 0
        self.waited = {e: {} for e in self.eng}
        self.lastw = {}
        self.readers = {}
        self.psacc = {}
        self.sb_off = SB_BASE
        self.limit = SB_TOP
        self.nalloc = 0
        self.ps = [nc.alloc_psum_tensor("psb%d" % i, [128, 512], F32) for i in range(8)]

    def sb(self, name, shape, dtype):
        esz = 2 if dtype == BF16 else 4
        n = 1
        for s in shape[1:]:
            n *= s
        nbytes = (n * esz + 31) // 32 * 32
        off = self.sb_off
        self.sb_off += nbytes
        assert self.sb_off <= self.limit, "SBUF overflow at %s: %d > %d" % (name, self.sb_off, self.limit)
        self.nalloc += 1
        return self.nc.alloc_sbuf_tensor_at("%s_%d" % (name, self.nalloc), list(shape), dtype, offset=off)

    def mark(self):
        return self.sb_off

    def release(self, m):
        self.sb_off = m

    def _deps(self, reads, writes):
        deps = []
        for k in reads:
            t = self.lastw.get(k)
            if t is not None:
                deps.append(t)
        for k in writes:
            t = self.lastw.get(k)
            if t is not None:
                deps.append(t)
            deps.extend(self.readers.get(k, {}).items())
        return deps

    def _emit_waits(self, e, deps, skip_own):
        need = {}
        own = 's_' + e
        for (s, v) in deps:
            if skip_own and s == own:
                continue
            if v > need.get(s, 0):
                need[s] = v
        w = self.waited[e]
        for s, v in need.items():
            if w.get(s, 0) >= v:
                continue
            self.eng[e].wait_ge(self.semh[s], v)
            w[s] = v

    def _commit(self, tok, reads, writes):
        s, v = tok
        for k in reads:
            self.readers.setdefault(k, {})[s] = v
        for k in writes:
            self.lastw[k] = tok
            self.readers[k] = {}

    def op(self, e, reads, writes, fn):
        deps = self._deps(reads, writes)
        s = 's_' + e
        banks = set(k[1] for k in list(reads) + list(writes) if isinstance(k, tuple) and k[0] == 'ps')
        for b in banks:
            for s2, v2 in self.psacc.get(b, {}).items():
                if s2 != s:
                    deps.append((s2, v2))
        self._emit_waits(e, deps, e == 'pe')
        inst = fn(self.eng[e])
        self.cnt[s] += 1
        inst.then_inc(self.semh[s], 1)
        self._commit((s, self.cnt[s]), reads, writes)
        for b in banks:
            self.psacc.setdefault(b, {})[s] = self.cnt[s]

    def dma(self, q, out, in_, reads, writes, semkey, **kw):
        self._emit_waits(q, self._deps(reads, writes), False)
        s = 'd_' + semkey
        if s not in self.semh:
            self.semh[s] = self.nc.alloc_semaphore(s)
            self.cnt[s] = 0
        inst = self.eng[q].dma_start(out=out, in_=in_, **kw)
        self.cnt[s] += 16
        inst.then_inc(self.semh[s], 16)
        self._commit((s, self.cnt[s]), reads, writes)

    def barrier(self):
        for e in self.eng:
            own = 's_' + e
            for s, v in self.cnt.items():
                if s == own or v == 0:
                    continue
                if self.waited[e].get(s, 0) >= v:
                    continue
                self.eng[e].wait_ge(self.semh[s], v)
                self.waited[e][s] = v
        self.lastw = {}
        self.readers = {}
        self.psacc = {}


def psb(c, i):
    return c.ps[i][:].bitcast(BF16)


class StopEmit(Exception):
    pass


def emit_layer(c, l, A, C, x_full, load_own, out_ap, final_norm, stop_after=None):
    nc = c.nc
    m_layer = c.mark()

    def stop_here(tag, dump=None):
        if stop_after != tag:
            return
        c.barrier()
        if dump is not None:
            dump()
        c.barrier()
        raise StopEmit()

    mixedT = c.sb("mixedT", [128, 8, 2048], BF16)
    m_c = c.mark()

    identb = c.sb("identb", [128, 128], BF16)
    identf = c.sb("identf", [128, 128], F32)
    trif = c.sb("trif", [128, 128], F32)
    onesf = c.sb("onesf", [128, 128], F32)
    shiftb = c.sb("shiftb", [32, 96], BF16)
    mmask = c.sb("mmask", [128, 4, 128], F32)
    smask = c.sb("smask", [128, 4, 128], F32)
    g1bc = c.sb("g1bc", [128, 1024], F32)
    gqbc = c.sb("gqbc", [128, 256], F32)
    gkvbc = c.sb("gkvbc", [128, 128], F32)
    Wuq = c.sb("Wuq", [128, 2, 768], BF16)
    Wrot = c.sb("Wrot", [128, 2, 768], BF16)
    Wukv = c.sb("Wukv", [128, 1024], BF16)
    WkPad = c.sb("WkPad", [128, 8, 96], BF16)
    poolw = c.sb("poolw", [64, 4, 64], BF16)
    pscale = c.sb("pscale", [64, 4], F32)

    c.dma('pool', identb[:], A['ident'], [], ['identb'], 'k1')
    c.dma('sp', identf[:], A['ident'], [], ['identf'], 'k2')
    c.dma('sp', trif[:], A['tri'], [], ['trif'], 'k3')
    c.dma('sp', onesf[:], A['ones'], [], ['onesf'], 'k4')
    c.dma('pool', shiftb[:], A['shift'], [], ['shiftb'], 'k5')
    c.dma('sp', mmask[:], C['mla_mask'].rearrange("m k q -> k m q"), [], ['mmask'], 'k6')
    c.dma('sp', smask[:], C['sb_mask'].rearrange("m k q -> k m q"), [], ['smask'], 'k7')
    c.dma('sp', g1bc[:], A['norm1_g'][l].partition_broadcast(128), [], ['g1bc'], 'k8')
    c.dma('sp', gqbc[:], A['q_norm_g'][l].partition_broadcast(128), [], ['gqbc'], 'k9')
    c.dma('sp', gkvbc[:], A['kv_norm_g'][l].partition_broadcast(128), [], ['gkvbc'], 'k10')
    c.dma('pool', Wuq[:], A['w_uq'][l].rearrange("(c p) f -> p c f", p=128), [], ['Wuq'], 'k11')
    c.dma('pool', Wukv[:], A['w_ukv'][l], [], ['Wukv'], 'k12')
    c.dma('pool', poolw[:], A['pool_w'][l].rearrange("g c d -> c g d"), [], ['poolw'], 'k13')
    c.dma('sp', pscale[:], A['pool_scale'][l].rearrange("(g c) -> c g", c=64), [], ['pscale'], 'k14',
          allow_slow_non_contiguous=True)

    c.op('pool', [], ['Wrot'], lambda e: e.memset(Wrot[:], 0.0))
    c.op('pool', [], ['WkPad'], lambda e: e.memset(WkPad[:], 0.0))
    for c2 in range(2):
        src = Wuq[:, c2, :].rearrange("p (h d) -> p h d", d=96)
        dst = Wrot[:, c2, :].rearrange("p (h d) -> p h d", d=96)
        c.op('pool', ['Wuq'], ['Wrot'],
             lambda e, s=src, d_=dst: e.tensor_scalar(out=d_[:, :, 64:80], in0=s[:, :, 80:96], scalar1=-1.0,
                                                      scalar2=None, op0=ALU.mult))
        c.op('pool', ['Wuq'], ['Wrot'],
             lambda e, s=src, d_=dst: e.tensor_copy(out=d_[:, :, 80:96], in_=s[:, :, 64:80]))
    wkv_v = Wukv[:].rearrange("p (h d) -> p h d", d=128)
    c.op('pool', ['Wukv'], ['WkPad'], lambda e: e.tensor_copy(out=WkPad[:, :, 0:64], in_=wkv_v[:, :, 0:64]))

    tmpO = [c.sb("tmpO", [64, 512], BF16) for _ in range(2)]
    cnt_m = {'n': 0}

    def write_mixed(hc16, J, eng, emit_fn, reads):
        ch, odd = hc16 // 2, hc16 % 2
        dst_cols = slice(J * 512, (J + 1) * 512)
        if not odd:
            c.op(eng, reads, [('mixedT', hc16, J)], lambda e: emit_fn(e, mixedT[0:64, ch, dst_cols]))
            return
        n = cnt_m['n']
        cnt_m['n'] += 1
        t_ = tmpO[n % 2]
        kt = ('tmpO', n % 2)
        c.op(eng, reads, [kt], lambda e: emit_fn(e, t_[:, :]))
        c.op('pe', [kt, 'shift64'], [('ps', 7)],
             lambda e: e.matmul(c.ps[7][:, :], lhsT=shift64[:, :], rhs=t_[:, :], start=True, stop=True))
        c.op('act', [('ps', 7)], [('mixedT', hc16, J)],
             lambda e: e.copy(out=mixedT[64:128, ch, dst_cols], in_=c.ps[7][64:128, :]))

    stop_here('A0', lambda: c.dma('sp', out_ap[0:128, :], g1bc[:], ['g1bc'], [], 'out'))
    ckvnT = c.sb("ckvnT", [128, 4096], BF16)
    kropeT = c.sb("kropeT", [32, 4096], BF16)
    cqnT = c.sb("cqnT", [128, 2, 2048], BF16)
    KbT = c.sb("KbT", [128, 2, 4096], BF16)
    QbT = c.sb("QbT", [128, 2, 2048], BF16)
    Vb = c.sb("Vb", [128, 32, 256], BF16)
    ub = c.sb("ub", [128, 32, 256], BF16)
    shift64 = c.sb("shift64", [64, 128], BF16)
    c.dma('pool', shift64[:], A['shift64'], [], ['shift64'], 'shift64')
    m_a = c.mark()

    Win = c.sb("Win", [128, 8, 1440], BF16)
    c.dma('pool', Win[:], A['w_in'][l].rearrange("(c p) f -> p c f", p=128), [], ['Win'], 'k15')
    xbuf = [c.sb("xbuf", [128, 1024], F32) for _ in range(3)]
    hnb = [c.sb("hnb", [128, 1024], BF16) for _ in range(2)]
    sq = c.sb("sq", [128, 1024], BF16)
    hnT = [c.sb("hnT", [128, 8, 512], BF16) for _ in range(2)]
    stat = c.sb("stat", [128, 64], F32)
    ckvn = [c.sb("ckvn", [128, 256], BF16) for _ in range(2)]
    krr = [c.sb("krr", [128, 32], BF16) for _ in range(2)]
    rt = c.sb("rt", [128, 4, 16], F32)
    cst = [c.sb("cst", [128, 4, 32], F32) for _ in range(2)]

    cnt = {'blk': 0, 'bat': 0}

    def front(loader, nb, hT):
        n = cnt['blk']
        cnt['blk'] += 1
        xb = xbuf[n % 3]
        kx = ('x', n % 3)
        loader(xb, kx, 'x%d' % (n % 3))
        so = (n % 8) * 4
        kst = ('st', n % 8)
        c.op('act', [kx], ['sq', kst],
             lambda e: e.activation(out=sq[:], in_=xb[:], func=AF.Square, accum_out=stat[:, so:so + 1]))
        c.op('act', [kst], [kst],
             lambda e: e.activation(out=stat[:, so + 1:so + 2], in_=stat[:, so:so + 1], func=AF.Sqrt,
                                    scale=1.0 / D, bias=EPS))
        c.op('dve', [kst], [kst], lambda e: e.reciprocal(out=stat[:, so + 2:so + 3], in_=stat[:, so + 1:so + 2]))
        hb = hnb[n % 2]
        kh = ('hn', n % 2)
        c.op('dve', [kx, kst, 'g1bc'], [kh],
             lambda e: e.scalar_tensor_tensor(out=hb[:], in0=xb[:], scalar=stat[:, so + 2:so + 3], in1=g1bc[:],
                                              op0=ALU.mult, op1=ALU.mult))
        bank = n % 2
        pv = psb(c, bank)

        def tr(e):
            inst = None
            for cc in range(8):
                inst = e.transpose(pv[:, cc * 128:(cc + 1) * 128], hb[:, cc * 128:(cc + 1) * 128], identb[:])
            return inst
        c.op('pe', [kh, 'identb'], [('ps', bank)], tr)
        ev = 'act' if n % 2 == 0 else 'dve'
        dst = hT[:, :, nb * 128:(nb + 1) * 128]
        srcv = pv[:, :].rearrange("p (c t) -> p c t", t=128)
        if ev == 'act':
            c.op('act', [('ps', bank)], [('hT', id(hT), nb)], lambda e: e.copy(out=dst, in_=srcv))
        else:
            c.op('dve', [('ps', bank)], [('hT', id(hT), nb)], lambda e: e.tensor_copy(out=dst, in_=srcv))

    def mm_acc(bank_ap, lhs_fn, rhs_fn, nk):
        def f(e):
            inst = None
            for cc in range(nk):
                inst = e.matmul(bank_ap, lhsT=lhs_fn(cc), rhs=rhs_fn(cc), start=(cc == 0), stop=(cc == nk - 1))
            return inst
        return f

    def kv_front(b, u):
        hT = hnT[u % 2]
        for nb in range(4):
            slot = b * 4 + nb
            front(lambda xb, kx, sk, slot=slot: c.dma('sp', xb[:], x_full[slot * 128:(slot + 1) * 128, :], [], [kx], sk), nb, hT)

    def kv_proj(b, u):
        hT = hnT[u % 2]
        hkeys = [('hT', id(hT), nb) for nb in range(4)]
        cs_t = cst[b % 2]
        c.dma('sp', cs_t[:], A['cs_full'][b * 512:(b + 1) * 512, :].rearrange("(n p) f -> p n f", p=128),
              [], [('cst', b % 2)], 'cst%d' % (b % 2))
        for nb in range(4):
            slot = b * 4 + nb
            n = cnt['bat']
            cnt['bat'] += 1
            ba = 2 + n % 2
            c.op('pe', [hkeys[nb], 'Win'], [('ps', ba)],
                 mm_acc(c.ps[ba][:, :], lambda cc: hT[:, cc, nb * 128:(nb + 1) * 128],
                        lambda cc: Win[:, cc, 928:1440], 8))
            c.op('act', [('ps', ba)], [('Vb', slot)], lambda e, ba=ba, slot=slot: e.copy(out=Vb[:, slot, :], in_=c.ps[ba][:, 0:256]))
            c.op('dve', [('ps', ba)], [('ub', slot)], lambda e, ba=ba, slot=slot: e.tensor_copy(out=ub[:, slot, :], in_=c.ps[ba][:, 256:512]))
            bb = 4 + n % 2
            c.op('pe', [hkeys[nb], 'Win'], [('ps', bb)],
                 mm_acc(c.ps[bb][:, 0:160], lambda cc: hT[:, cc, nb * 128:(nb + 1) * 128],
                        lambda cc: Win[:, cc, 256:416], 8))
            so = 32 + (n % 8) * 4
            kst = ('st2', n % 8)
            c.op('act', [('ps', bb)], ['sq', kst],
                 lambda e, bb=bb, so=so: e.activation(out=sq[:, 0:128], in_=c.ps[bb][:, 0:128], func=AF.Square,
                                                      accum_out=stat[:, so:so + 1]))
            c.op('act', [kst], [kst],
                 lambda e, so=so: e.activation(out=stat[:, so + 1:so + 2], in_=stat[:, so:so + 1], func=AF.Sqrt,
                                               scale=1.0 / 128, bias=EPS))
            c.op('dve', [kst], [kst],
                 lambda e, so=so: e.reciprocal(out=stat[:, so + 2:so + 3], in_=stat[:, so + 1:so + 2]))
            ck = ckvn[n % 2]
            kck = ('ckvn', n % 2)
            c.op('dve', [('ps', bb), kst, 'gkvbc'], [kck],
                 lambda e, bb=bb, so=so, ck=ck: e.scalar_tensor_tensor(
                     out=ck[:, 0:128], in0=c.ps[bb][:, 0:128], scalar=stat[:, so + 2:so + 3], in1=gkvbc[:],
                     op0=ALU.mult, op1=ALU.mult))
            kr = krr[n % 2]
            kkr = ('krr', n % 2)
            x1 = c.ps[bb][:, 128:144]
            x2 = c.ps[bb][:, 144:160]
            cos = cs_t[:, nb, 0:16]
            sin = cs_t[:, nb, 16:32]
            kcs = ('cst', b % 2)
            c.op('dve', [('ps', bb), kcs], ['rt0'], lambda e, x1=x1, cos=cos: e.tensor_tensor(out=rt[:, 0, :], in0=x1, in1=cos, op=ALU.mult))
            c.op('dve', [('ps', bb), kcs], ['rt1'], lambda e, x2=x2, sin=sin: e.tensor_tensor(out=rt[:, 1, :], in0=x2, in1=sin, op=ALU.mult))
            c.op('dve', [('ps', bb), kcs], ['rt2'], lambda e, x2=x2, cos=cos: e.tensor_tensor(out=rt[:, 2, :], in0=x2, in1=cos, op=ALU.mult))
            c.op('dve', [('ps', bb), kcs], ['rt3'], lambda e, x1=x1, sin=sin: e.tensor_tensor(out=rt[:, 3, :], in0=x1, in1=sin, op=ALU.mult))
            c.op('dve', ['rt0', 'rt1'], [kkr], lambda e, kr=kr: e.tensor_tensor(out=kr[:, 0:16], in0=rt[:, 0, :], in1=rt[:, 1, :], op=ALU.subtract))
            c.op('dve', ['rt2', 'rt3'], [kkr], lambda e, kr=kr: e.tensor_tensor(out=kr[:, 16:32], in0=rt[:, 2, :], in1=rt[:, 3, :], op=ALU.add))
            pv6 = psb(c, 6)

            def tr2(e, ck=ck, kr=kr, nb=nb):
                e.transpose(pv6[:, nb * 128:(nb + 1) * 128], ck[:, 0:128], identb[:])
                return e.transpose(pv6[0:32, 512 + nb * 128:512 + (nb + 1) * 128], kr[:, :], identb[:])
            c.op('pe', [kck, kkr, 'identb'], [('ps', 6)], tr2)
        pv6 = psb(c, 6)
        c.op('dve', [('ps', 6)], [('ckvnT', b)], lambda e, b=b: e.tensor_copy(out=ckvnT[:, b * 512:(b + 1) * 512], in_=pv6[:, 0:512]))
        c.op('dve', [('ps', 6)], [('kropeT', b)], lambda e, b=b: e.tensor_copy(out=kropeT[:, b * 512:(b + 1) * 512], in_=pv6[0:32, 512:1024]))
        for hc in range(2):
            c.op('pe', hkeys + ['Win'], [('ps', 7)],
                 mm_acc(c.ps[7][:, :], lambda cc, hc=hc: Win[:, cc, 672 + hc * 128:672 + (hc + 1) * 128],
                        lambda cc: hT[:, cc, :], 8))
            if hc == 0:
                c.op('act', [('ps', 7)], [('KbT', b, hc)], lambda e, b=b, hc=hc: e.copy(out=KbT[:, hc, b * 512:(b + 1) * 512], in_=c.ps[7][:, :]))
            else:
                c.op('dve', [('ps', 7)], [('KbT', b, hc)], lambda e, b=b, hc=hc: e.tensor_copy(out=KbT[:, hc, b * 512:(b + 1) * 512], in_=c.ps[7][:, :]))

    def own_front(ob, u):
        hT = hnT[u % 2]
        for nb in range(4):
            j = ob * 4 + nb
            front(lambda xb, kx, sk, j=j: load_own(j, xb, kx, sk), nb, hT)

    def own_proj(ob, u):
        hT = hnT[u % 2]
        hkeys = [('hT', id(hT), nb) for nb in range(4)]
        for nb in range(4):
            n = cnt['bat']
            cnt['bat'] += 1
            bb = 4 + n % 2
            c.op('pe', [hkeys[nb], 'Win'], [('ps', bb)],
                 mm_acc(c.ps[bb][:, 0:256], lambda cc: hT[:, cc, nb * 128:(nb + 1) * 128],
                        lambda cc: Win[:, cc, 0:256], 8))
            so = 32 + (n % 8) * 4
            kst = ('st2', n % 8)
            c.op('act', [('ps', bb)], ['sq', kst],
                 lambda e, bb=bb, so=so: e.activation(out=sq[:, 0:256], in_=c.ps[bb][:, 0:256], func=AF.Square,
                                                      accum_out=stat[:, so:so + 1]))
            c.op('act', [kst], [kst],
                 lambda e, so=so: e.activation(out=stat[:, so + 1:so + 2], in_=stat[:, so:so + 1], func=AF.Sqrt,
                                               scale=1.0 / 256, bias=EPS))
            c.op('dve', [kst], [kst],
                 lambda e, so=so: e.reciprocal(out=stat[:, so + 2:so + 3], in_=stat[:, so + 1:so + 2]))
            ck = ckvn[n % 2]
            kck = ('ckvn', n % 2)
            c.op('dve', [('ps', bb), kst, 'gqbc'], [kck],
                 lambda e, bb=bb, so=so, ck=ck: e.scalar_tensor_tensor(
                     out=ck[:, :], in0=c.ps[bb][:, 0:256], scalar=stat[:, so + 2:so + 3], in1=gqbc[:],
                     op0=ALU.mult, op1=ALU.mult))
            pv6 = psb(c, 6)

            def tr3(e, ck=ck, nb=nb):
                e.transpose(pv6[:, nb * 128:(nb + 1) * 128], ck[:, 0:128], identb[:])
                return e.transpose(pv6[:, 512 + nb * 128:512 + (nb + 1) * 128], ck[:, 128:256], identb[:])
            c.op('pe', [kck, 'identb'], [('ps', 6)], tr3)
        pv6 = psb(c, 6)
        c.op('dve', [('ps', 6)], [('cqnT', ob)],
             lambda e, ob=ob: e.tensor_copy(out=cqnT[:, :, ob * 512:(ob + 1) * 512],
                                            in_=pv6[:, :].rearrange("p (c t) -> p c t", t=512)))
        for hc in range(2):
            c.op('pe', hkeys + ['Win'], [('ps', 7)],
                 mm_acc(c.ps[7][:, :], lambda cc, hc=hc: Win[:, cc, 416 + hc * 128:416 + (hc + 1) * 128],
                        lambda cc: hT[:, cc, :], 8))
            if hc == 0:
                c.op('act', [('ps', 7)], [('QbT', ob, hc)], lambda e, ob=ob, hc=hc: e.copy(out=QbT[:, hc, ob * 512:(ob + 1) * 512], in_=c.ps[7][:, :]))
            else:
                c.op('dve', [('ps', 7)], [('QbT', ob, hc)], lambda e, ob=ob, hc=hc: e.tensor_copy(out=QbT[:, hc, ob * 512:(ob + 1) * 512], in_=c.ps[7][:, :]))

    units = [(kv_front, kv_proj, b) for b in range(8)] + [(own_front, own_proj, ob) for ob in range(4)]
    units[0][0](units[0][2], 0)
    for u, (ff, pf, arg) in enumerate(units):
        if u + 1 < len(units):
            units[u + 1][0](units[u + 1][2], u + 1)
        pf(arg, u)

    c.barrier()
    c.release(m_a)
    stop_here('A', None)

    m_p = c.mark()
    bands = c.sb("bands", [128, 48, 128], BF16)
    dT = c.sb("dT", [64, 4, 512], BF16)
    c.dma('pool', bands[:], C['bands'].rearrange("m s t -> s m t"), [], ['bands'], 'k16')
    for ob in range(4):
        for g in range(4):
            def pm(e, ob=ob, g=g):
                inst = None
                for nb in range(4):
                    j = ob * 4 + nb
                    var = 2 if j == 0 else j % 2
                    srcs = [(0, j), (1, j)]
                    if j > 0:
                        srcs += [(0, j - 1), (1, j - 1)]
                    for wi, (r, i) in enumerate(srcs):
                        slot = r * 16 + i
                        inst = e.matmul(c.ps[g][0:64, nb * 128:(nb + 1) * 128],
                                        lhsT=ub[:, slot, g * 64:(g + 1) * 64],
                                        rhs=bands[:, (wi * 3 + var) * 4 + g, :],
                                        start=(wi == 0), stop=(wi == len(srcs) - 1))
                return inst
            c.op('pe', ['bands'], [('ps', g)], pm)
            c.op('act', [('ps', g)], [('dT', g)], lambda e, g=g: e.copy(out=dT[:, g, :], in_=c.ps[g][0:64, :]))
            c.op('pe', [('dT', g), 'poolw'], [('ps', 4 + g % 2)],
                 lambda e, g=g: e.matmul(c.ps[4 + g % 2][0:64, :], lhsT=poolw[:, g, :], rhs=dT[:, g, :], start=True, stop=True))
            write_mixed(12 + g, ob, 'dve',
                        lambda e, o, g=g: e.tensor_scalar(out=o, in0=c.ps[4 + g % 2][0:64, :], scalar1=pscale[:, g:g + 1],
                                                          scalar2=None, op0=ALU.mult),
                        [('ps', 4 + g % 2), 'pscale'])
    c.barrier()
    c.release(m_p)
    stop_here('P', None)

    m_b = c.mark()
    KhT = c.sb("KhT", [128, 4096], BF16)
    Vh = c.sb("Vh", [128, 32, 65], BF16)
    QhT = c.sb("QhT", [128, 2048], BF16)
    csT = [c.sb("csT", [128, 2, 512], F32) for _ in range(2)]
    rq = [c.sb("rq", [128, 2, 512], F32) for _ in range(2)]
    pT = [c.sb("pT", [128, 512], BF16) for _ in range(5)]
    SBK = [0, 1, 4, 5]
    osb = [c.sb("osb", [128, 512], F32) for _ in range(2)]
    rec = c.sb("rec", [128, 512], F32)
    c.op('pool', [], ['Vh1'], lambda e: e.memset(Vh[:, :, 64:65], 1.0))
    SCALE_A = 96.0 ** -0.5
    cnt['q'] = 0
    cnt['s'] = 0
    cnt['o'] = 0
    pend_tail = []

    for h in range(A_HEADS):
        if pend_tail:
            pend_tail.pop()()
        for b in range(8):
            bank = b % 2

            def kb(e, b=b, bank=bank, h=h):
                e.matmul(c.ps[bank][0:96, :], lhsT=WkPad[:, h, :], rhs=ckvnT[:, b * 512:(b + 1) * 512], start=True, stop=False)
                return e.matmul(c.ps[bank][0:96, :], lhsT=shiftb[:, :], rhs=kropeT[:, b * 512:(b + 1) * 512], start=False, stop=True)
            c.op('pe', ['WkPad', 'shiftb', ('ckvnT', b), ('kropeT', b)], [('ps', bank)], kb)
            if b % 2 == 0:
                c.op('act', [('ps', bank)], [('KhT', b)], lambda e, b=b, bank=bank: e.copy(out=KhT[0:96, b * 512:(b + 1) * 512], in_=c.ps[bank][0:96, :]))
            else:
                c.op('dve', [('ps', bank)], [('KhT', b)], lambda e, b=b, bank=bank: e.tensor_copy(out=KhT[0:96, b * 512:(b + 1) * 512], in_=c.ps[bank][0:96, :]))
        for gq in range(4):
            bank = 2 + gq % 2

            def vb(e, gq=gq, bank=bank, h=h):
                inst = None
                for s8 in range(8):
                    slot = gq * 8 + s8
                    inst = e.matmul(c.ps[bank][:, s8 * 64:(s8 + 1) * 64], lhsT=ckvnT[:, slot * 128:(slot + 1) * 128],
                                    rhs=wkv_v[:, h, 64:128], start=True, stop=True)
                return inst
            c.op('pe', ['Wukv'] + [('ckvnT', b) for b in (2 * gq, 2 * gq + 1)], [('ps', bank)], vb)
            ev = 'act' if gq % 2 == 0 else 'dve'
            dst = Vh[:, gq * 8:(gq + 1) * 8, 0:64]
            srcv = c.ps[bank][:, :].rearrange("p (s d) -> p s d", d=64)
            if ev == 'act':
                c.op('act', [('ps', bank)], [('Vh', gq)], lambda e, dst=dst, srcv=srcv: e.copy(out=dst, in_=srcv))
            else:
                c.op('dve', [('ps', bank)], [('Vh', gq)], lambda e, dst=dst, srcv=srcv: e.tensor_copy(out=dst, in_=srcv))
        for ob in range(4):
            nq = cnt['q']
            cnt['q'] += 1
            ct = csT[nq % 2]
            kct = ('csT', nq % 2)
            c.dma('sp', ct[64:96, 0, :], C['cs_own'][0:32, ob * 512:(ob + 1) * 512], [], [kct], 'csT%d' % (nq % 2))
            c.dma('sp', ct[64:96, 1, :], C['cs_own'][32:64, ob * 512:(ob + 1) * 512], [], [kct], 'csT%d' % (nq % 2))

            def qb(e, ob=ob, h=h):
                e.matmul(c.ps[4][0:96, :], lhsT=Wuq[:, 0, h * 96:(h + 1) * 96], rhs=cqnT[:, 0, ob * 512:(ob + 1) * 512], start=True, stop=False)
                e.matmul(c.ps[4][0:96, :], lhsT=Wuq[:, 1, h * 96:(h + 1) * 96], rhs=cqnT[:, 1, ob * 512:(ob + 1) * 512], start=False, stop=True)
                e.matmul(c.ps[5][0:96, :], lhsT=Wrot[:, 0, h * 96:(h + 1) * 96], rhs=cqnT[:, 0, ob * 512:(ob + 1) * 512], start=True, stop=False)
                return e.matmul(c.ps[5][0:96, :], lhsT=Wrot[:, 1, h * 96:(h + 1) * 96], rhs=cqnT[:, 1, ob * 512:(ob + 1) * 512], start=False, stop=True)
            c.op('pe', ['Wuq', 'Wrot', ('cqnT', ob)], [('ps', 4), ('ps', 5)], qb)
            c.op('act', [('ps', 4)], [('QhT', ob)], lambda e, ob=ob: e.copy(out=QhT[0:64, ob * 512:(ob + 1) * 512], in_=c.ps[4][0:64, :]))
            r_ = rq[nq % 2]
            krq = ('rq', nq % 2)
            c.op('dve', [('ps', 4), kct], [krq], lambda e, r_=r_, ct=ct: e.tensor_tensor(out=r_[64:96, 0, :], in0=c.ps[4][64:96, :], in1=ct[64:96, 0, :], op=ALU.mult))
            c.op('dve', [('ps', 5), kct], [krq], lambda e, r_=r_, ct=ct: e.tensor_tensor(out=r_[64:96, 1, :], in0=c.ps[5][64:96, :], in1=ct[64:96, 1, :], op=ALU.mult))
            c.op('dve', [krq], [('QhT', ob)], lambda e, r_=r_, ob=ob: e.tensor_tensor(out=QhT[64:96, ob * 512:(ob + 1) * 512], in0=r_[64:96, 0, :], in1=r_[64:96, 1, :], op=ALU.add))
        for J in range(4):
            j0 = 4 * J
            no = cnt['o']
            cnt['o'] += 1
            obank = 2 + no % 2
            klist = [(r, i) for i in range(j0 + 4) for r in range(2)]
            nk = len(klist)
            base = cnt['s']
            cnt['s'] += nk

            def S_(n, klist=klist, j0=j0, J=J, base=base):
                r, i = klist[n]
                slot = r * 16 + i
                c0 = max(0, i - j0) * 128
                sbank = SBK[(base + n) % 4]
                c.op('pe', [('KhT', slot // 4), ('QhT', J)], [('ps', sbank)],
                     lambda e: e.matmul(c.ps[sbank][:, c0:512], lhsT=KhT[0:96, slot * 128:(slot + 1) * 128],
                                        rhs=QhT[0:96, J * 512 + c0:(J + 1) * 512], start=True, stop=True))

            def E_(n, klist=klist, j0=j0, base=base):
                r, i = klist[n]
                c0 = max(0, i - j0) * 128
                sbank = SBK[(base + n) % 4]
                p_ = pT[(base + n) % 5]
                kp = ('pT', (base + n) % 5)
                c.op('act', [('ps', sbank)], [kp],
                     lambda e: e.activation(out=p_[:, c0:512], in_=c.ps[sbank][:, c0:512], func=AF.Exp, scale=SCALE_A))
                if i >= j0:
                    mi = r * 2 + (i % 2)
                    c.op('pool', [kp, 'mmask'], [kp],
                         lambda e: e.tensor_tensor(out=p_[:, c0:c0 + 128], in0=p_[:, c0:c0 + 128], in1=mmask[:, mi, :], op=ALU.mult))

            def PV_(n, klist=klist, j0=j0, base=base, obank=obank, nk=nk):
                r, i = klist[n]
                slot = r * 16 + i
                c0 = max(0, i - j0) * 128
                p_ = pT[(base + n) % 5]
                kp = ('pT', (base + n) % 5)
                c.op('pe', [kp, ('Vh', slot // 8), 'Vh1'], [('ps', obank)],
                     lambda e: e.matmul(c.ps[obank][0:65, c0:512], lhsT=Vh[:, slot, :], rhs=p_[:, c0:512],
                                        start=(n == 0), stop=(n == nk - 1)))

            def tail(no=no, obank=obank, h=h, J=J):
                o_ = osb[no % 2]
                ko = ('osb', no % 2)
                c.op('act', [('ps', obank)], [ko], lambda e: e.copy(out=o_[0:65, :], in_=c.ps[obank][0:65, :]))
                c.op('pe', [ko, 'onesf'], [('ps', 6)],
                     lambda e: e.matmul(c.ps[6][0:64, :], lhsT=onesf[64:65, 0:64], rhs=o_[64:65, :], start=True, stop=True))
                c.op('dve', [('ps', 6)], ['rec'], lambda e: e.reciprocal(out=rec[0:64, :], in_=c.ps[6][0:64, :]))
                write_mixed(h, J, 'dve',
                            lambda e, o: e.tensor_tensor(out=o, in0=o_[0:64, :], in1=rec[0:64, :], op=ALU.mult),
                            [ko, 'rec'])
            for n in range(min(4, nk)):
                S_(n)
            if pend_tail:
                pend_tail.pop()()
            for n in range(nk):
                E_(n)
                PV_(n)
                if n + 4 < nk:
                    S_(n + 4)
            pend_tail.append(tail)
    if pend_tail:
        pend_tail.pop()()

    c.barrier()
    stop_here('B1', None)
    ebuf = [c.sb("ebuf", [128, 512], F32) for _ in range(2)]
    spb = [c.sb("spb", [128, 512], F32) for _ in range(3)]
    sph = [c.sb("sph", [128, 512], BF16) for _ in range(3)]
    Lacc = c.sb("Lacc", [128, 512], F32)
    Lbf = [c.sb("Lbf", [128, 512], BF16) for _ in range(2)]
    QbTn = c.sb("QbTn", [128, 2048], BF16)
    triib = c.sb("triib", [128, 128], BF16)
    onesb = c.sb("onesb", [128, 128], BF16)
    c.dma('pool', triib[:], A['trii'], [], ['triib'], 'triib')
    c.dma('pool', onesb[:], A['ones'], [], ['onesb'], 'onesb')
    SCALE_B = 64.0 ** -0.5
    cnt['z'] = 0
    for hb in range(B_HEADS):
        pb = (hb % 2) * 64
        hc = hb // 2
        c.op('dve', [], ['QbTn'], lambda e: e.memset(QbTn[:], 0.0))
        c.op('act', [], ['QbTn'],
             lambda e, pb=pb, hc=hc: e.activation(out=QbTn[pb:pb + 64, :], in_=QbT[pb:pb + 64, hc, :], func=AF.Copy,
                                                scale=-SCALE_B))
        for J in range(4):
            j0 = 4 * J
            no = cnt['o']
            cnt['o'] += 1
            obank = 4 + no % 2
            c.op('dve', [], ['Lacc'], lambda e: e.memset(Lacc[:], 0.0))
            klist = []
            for i in range(j0 + 3, -1, -1):
                klist += [(1, i), (0, i)] if i % 2 == 0 else [(0, i), (1, i)]
            nk = len(klist)
            base = cnt['z']
            cnt['z'] += nk

            def info(n, klist=klist, j0=j0, base=base):
                r, i = klist[n]
                g = base + n
                return dict(r=r, i=i, slot=r * 16 + i, c0=max(0, i - j0) * 128, xb=g % 4,
                            e_=ebuf[g % 2], s_=spb[g % 3], sh_=sph[g % 3], a_=pT[g % 5],
                            lb_=Lbf[g % 2], lbn_=Lbf[(g + 1) % 2],
                            ke=('eb', g % 2), ks=('sp', g % 3), ksh=('sph', g % 3), ka=('pT', g % 5),
                            klb=('Lbf', g % 2), klbn=('Lbf', (g + 1) % 2),
                            mi=r * 2 + (i % 2), masked=(i >= j0))

            def stage1(n, info=info, J=J, hc=hc):
                d = info(n)
                c0, xb, slot = d['c0'], d['xb'], d['slot']
                e_, s_, sh_ = d['e_'], d['s_'], d['sh_']
                c.op('pe', [('KbT', slot // 4, hc), 'QbTn'], [('ps', xb)],
                     lambda e: e.matmul(c.ps[xb][:, c0:512], lhsT=KbT[:, hc, slot * 128:(slot + 1) * 128],
                                        rhs=QbTn[:, J * 512 + c0:(J + 1) * 512], start=True, stop=True))
                c.op('act', [('ps', xb)], [d['ke']],
                     lambda e: e.activation(out=e_[:, c0:512], in_=c.ps[xb][:, c0:512], func=AF.Exp, scale=-1.0))
                c.op('act', [d['ke']], [d['ks']],
                     lambda e: e.activation(out=s_[:, c0:512], in_=e_[:, c0:512], func=AF.Ln, bias=1.0, scale=1.0))
                if d['masked']:
                    mi = d['mi']
                    c.op('pool', [d['ks'], 'smask'], [d['ks']],
                         lambda e: e.tensor_tensor(out=s_[:, c0:c0 + 128], in0=s_[:, c0:c0 + 128], in1=smask[:, mi, :], op=ALU.mult))
                c.op('dve', [d['ks']], [d['ksh']], lambda e: e.tensor_copy(out=sh_[:, c0:512], in_=s_[:, c0:512]))

            def stage2a(n, info=info, nk=nk):
                d = info(n)
                c0, xb = d['c0'], d['xb']
                s_, sh_, lb_, lbn_ = d['s_'], d['sh_'], d['lb_'], d['lbn_']

                def cum(e):
                    inst = e.matmul(c.ps[xb][:, c0:512], lhsT=triib[:, :], rhs=sh_[:, c0:512], start=False, stop=(n == 0),
                                    skip_group_check=True)
                    if n > 0:
                        inst = e.matmul(c.ps[xb][:, c0:512], lhsT=onesb[:, :], rhs=lb_[:, c0:512], start=False, stop=True,
                                        skip_group_check=True)
                    return inst
                rd = [d['ksh'], 'triib', 'onesb']
                if n > 0:
                    rd.append(d['klb'])
                c.op('pe', rd, [('ps', xb)], cum)
                if n < nk - 1:
                    c.op('dve', [d['ks'], 'Lacc'], ['Lacc'],
                         lambda e: e.tensor_tensor(out=Lacc[:, c0:512], in0=Lacc[:, c0:512], in1=s_[:, c0:512], op=ALU.add))
                    c.op('dve', ['Lacc'], [d['klbn']], lambda e: e.tensor_copy(out=lbn_[:, :], in_=Lacc[:, :]))

            def stage2b(n, info=info):
                d = info(n)
                c0, xb, a_ = d['c0'], d['xb'], d['a_']
                c.op('act', [('ps', xb)], [d['ka']],
                     lambda e: e.activation(out=a_[:, c0:512], in_=c.ps[xb][:, c0:512], func=AF.Exp, scale=-1.0))
                if d['masked']:
                    mi = d['mi']
                    c.op('pool', [d['ka'], 'smask'], [d['ka']],
                         lambda e: e.tensor_tensor(out=a_[:, c0:c0 + 128], in0=a_[:, c0:c0 + 128], in1=smask[:, mi, :], op=ALU.mult))

            def pv(n, info=info, nk=nk, obank=obank, hb=hb):
                d = info(n)
                c0, slot, a_ = d['c0'], d['slot'], d['a_']
                c.op('pe', [d['ka']], [('ps', obank)],
                     lambda e: e.matmul(c.ps[obank][0:64, c0:512], lhsT=Vb[:, slot, hb * 64:(hb + 1) * 64], rhs=a_[:, c0:512],
                                        start=(n == 0), stop=(n == nk - 1), skip_group_check=True))
            stage1(0)
            stage1(1)
            for n in range(nk):
                stage2a(n)
                if n + 2 < nk:
                    stage1(n + 2)
                stage2b(n)
                if n >= 1:
                    pv(n - 1)
            pv(nk - 1)
            write_mixed(8 + hb, J, 'dve',
                        lambda e, o, obank=obank: e.tensor_copy(out=o, in_=c.ps[obank][0:64, :]),
                        [('ps', obank)])

    c.barrier()
    c.release(m_c)

    XMID_OFF = SB_TOP - 16 * 1024 * 4
    c.nalloc += 1
    xmid = nc.alloc_sbuf_tensor_at("xmid_%d" % c.nalloc, [128, 16, 1024], F32, offset=XMID_OFF)
    c.limit = XMID_OFF
    Wout = c.sb("Wout", [128, 8, 1024], BF16)
    c.dma('pool', Wout[:], A['w_out'][l].rearrange("(hc p) d -> p hc d", p=128), [], ['Wout'], 'Wout')
    xo = [c.sb("xo", [128, 1024], F32) for _ in range(2)]
    for j in range(NOWN):
        xb = xo[j % 2]
        kx = ('xo', j % 2)
        load_own(j, xb, kx, 'xo%d' % (j % 2))
        for half in range(2):
            bank = (j * 2 + half) % 4

            def om(e, bank=bank, j=j, half=half):
                inst = None
                for hc in range(8):
                    inst = e.matmul(c.ps[bank][:, :], lhsT=mixedT[:, hc, j * 128:(j + 1) * 128],
                                    rhs=Wout[:, hc, half * 512:(half + 1) * 512], start=(hc == 0), stop=(hc == 7))
                return inst
            c.op('pe', ['Wout'], [('ps', bank)], om)
            c.op('dve', [('ps', bank), kx], [('xmid', j)],
                 lambda e, bank=bank, j=j, half=half, xb=xb: e.tensor_tensor(
                     out=xmid[:, j, half * 512:(half + 1) * 512], in0=c.ps[bank][:, :], in1=xb[:, half * 512:(half + 1) * 512], op=ALU.add))
    c.barrier()
    c.release(m_layer)

    if stop_after == 'C':
        for j in range(NOWN):
            c.dma('sp', out_ap[j * 128:(j + 1) * 128, :], xmid[:, j, :], [('xmid', j)], [], 'out')
        c.barrier()
        c.limit = SB_TOP
        return

    emit_moe(c, l, A, xmid, out_ap, final_norm)
    c.release(m_layer)
    c.limit = SB_TOP


def emit_moe(c, l, A, xmid, out_ap, final_norm):
    nc = c.nc
    m0 = c.mark()
    h2T = c.sb("h2T", [128, 8, 2048], BF16)
    cThi = c.sb("cThi", [32, 2048], BF16)
    cTlo = c.sb("cTlo", [32, 2048], BF16)
    zsel = c.sb("zsel", [32, 4096], BF16)
    identf = c.sb("identf2", [128, 128], F32)
    c.dma('pool', zsel[:], A['zsel'], [], ['zsel'], max_dma_last_dim=4096, semkey='k18')
    c.dma('sp', identf[:], A['ident'], [], ['identf'], 'k19')
    m1 = c.mark()
    g2bc = c.sb("g2bc", [128, 1024], F32)
    h2f = [c.sb("h2f", [128, 1024], F32) for _ in range(2)]
    h2T32 = [c.sb("h2T32", [128, 8, 128], F32) for _ in range(2)]
    Wr = c.sb("Wr", [128, 8, 36], F32)
    brbc = c.sb("brbc", [128, 36], F32)
    sq = c.sb("sq2", [128, 1024], BF16)
    st = c.sb("st2", [128, 16 * 4], F32)
    rb = [c.sb("rb", [128, 160], F32) for _ in range(2)]
    cTf = [c.sb("cTf", [32, 128], F32) for _ in range(2)]
    cTr = [c.sb("cTr", [32, 128], F32) for _ in range(2)]
    c.dma('sp', g2bc[:], A['norm2_g'][l].partition_broadcast(128), [], ['g2bc'], 'k20')
    c.dma('sp', Wr[:, :, 0:4], A['w_group'][l].rearrange("(c p) f -> p c f", p=128), [], ['Wr_g'], 'k21',
          allow_slow_non_contiguous=True)
    c.dma('sp', Wr[:, :, 4:36], A['w_expert'][l].rearrange("(c p) f -> p c f", p=128), [], ['Wr_e'], 'k22',
          allow_slow_non_contiguous=True)
    c.dma('sp', brbc[:, 0:4], A['b_group'][l].partition_broadcast(128), [], ['brbc_g'], 'k23')
    c.dma('sp', brbc[:, 4:36], A['b_expert'][l].partition_broadcast(128), [], ['brbc_e'], 'k24')

    for j in range(NOWN):
        so = j * 4
        kst = ('st', j)
        xj = xmid[:, j, :]
        c.op('act', [('xmid', j)], ['sq', kst],
             lambda e, xj=xj, so=so: e.activation(out=sq[:], in_=xj, func=AF.Square, accum_out=st[:, so:so + 1]))
        c.op('act', [kst], [kst],
             lambda e, so=so: e.activation(out=st[:, so + 1:so + 2], in_=st[:, so:so + 1], func=AF.Sqrt, scale=1.0 / D, bias=EPS))
        c.op('dve', [kst], [kst], lambda e, so=so: e.reciprocal(out=st[:, so + 2:so + 3], in_=st[:, so + 1:so + 2]))
        hf = h2f[j % 2]
        kh = ('h2f', j % 2)
        c.op('dve', [('xmid', j), kst, 'g2bc'], [kh],
             lambda e, hf=hf, xj=xj, so=so: e.scalar_tensor_tensor(out=hf[:], in0=xj, scalar=st[:, so + 2:so + 3], in1=g2bc[:],
                                                                  op0=ALU.mult, op1=ALU.mult))
        for hf2 in range(2):
            bank = hf2

            def tr(e, hf=hf, hf2=hf2, bank=bank):
                inst = None
                for q in range(4):
                    cc = hf2 * 4 + q
                    inst = e.transpose(c.ps[bank][:, q * 128:(q + 1) * 128], hf[:, cc * 128:(cc + 1) * 128], identf[:])
                return inst
            c.op('pe', [kh, 'identf'], [('ps', bank)], tr)
            t32 = h2T32[j % 2]
            k32 = ('h2T32', j % 2, hf2)
            srcv = c.ps[bank][:, :].rearrange("p (c t) -> p c t", t=128)
            c.op('act', [('ps', bank)], [k32], lambda e, t32=t32, hf2=hf2, srcv=srcv: e.copy(out=t32[:, hf2 * 4:(hf2 + 1) * 4, :], in_=srcv))
            c.op('dve', [('ps', bank)], [('h2T', j)],
                 lambda e, j=j, hf2=hf2, srcv=srcv: e.tensor_copy(out=h2T[:, hf2 * 4:(hf2 + 1) * 4, j * 128:(j + 1) * 128], in_=srcv))
        t32 = h2T32[j % 2]

        def rmm(e, t32=t32):
            inst = None
            for cc in range(8):
                inst = e.matmul(c.ps[2][:, 0:36], lhsT=t32[:, cc, :], rhs=Wr[:, cc, :], start=(cc == 0), stop=(cc == 7))
            return inst
        c.op('pe', [('h2T32', j % 2, 0), ('h2T32', j % 2, 1), 'Wr_g', 'Wr_e'], [('ps', 2)], rmm)
        r_ = rb[j % 2]
        kr = ('rb', j % 2)
        c.op('dve', [('ps', 2), 'brbc_g', 'brbc_e'], [kr], lambda e, r_=r_: e.tensor_tensor(out=r_[:, 0:36], in0=c.ps[2][:, 0:36], in1=brbc[:], op=ALU.add))
        c.op('dve', [kr], [kr], lambda e, r_=r_: e.tensor_reduce(out=r_[:, 40:41], in_=r_[:, 0:4], axis=mybir.AxisListType.X, op=ALU.max))
        c.op('dve', [kr], [kr], lambda e, r_=r_: e.tensor_scalar(out=r_[:, 36:40], in0=r_[:, 0:4], scalar1=r_[:, 40:41], scalar2=None, op0=ALU.is_ge))
        c.op('dve', [kr], [kr], lambda e, r_=r_: e.tensor_scalar(out=r_[:, 41:42], in0=r_[:, 40:41], scalar1=-1.0, scalar2=None, op0=ALU.mult))
        c.op('act', [kr], [kr], lambda e, r_=r_: e.activation(out=r_[:, 44:48], in_=r_[:, 0:4], func=AF.Exp, bias=r_[:, 41:42], scale=1.0, accum_out=r_[:, 48:49]))
        c.op('dve', [kr], [kr], lambda e, r_=r_: e.reciprocal(out=r_[:, 49:50], in_=r_[:, 48:49]))
        c.op('dve', [kr], [kr], lambda e, r_=r_: e.tensor_scalar(out=r_[:, 52:60], in0=r_[:, 4:12], scalar1=r_[:, 36:37], scalar2=None, op0=ALU.mult))
        for g in range(1, 4):
            c.op('dve', [kr], [kr], lambda e, r_=r_, g=g: e.scalar_tensor_tensor(
                out=r_[:, 52:60], in0=r_[:, 4 + 8 * g:12 + 8 * g], scalar=r_[:, 36 + g:37 + g], in1=r_[:, 52:60], op0=ALU.mult, op1=ALU.add))
        c.op('dve', [kr], [kr], lambda e, r_=r_: e.tensor_reduce(out=r_[:, 60:61], in_=r_[:, 52:60], axis=mybir.AxisListType.X, op=ALU.max))
        c.op('dve', [kr], [kr], lambda e, r_=r_: e.tensor_scalar(out=r_[:, 61:62], in0=r_[:, 60:61], scalar1=-1.0, scalar2=None, op0=ALU.mult))
        c.op('act', [kr], [kr], lambda e, r_=r_: e.activation(out=r_[:, 64:72], in_=r_[:, 52:60], func=AF.Exp, bias=r_[:, 61:62], scale=1.0))
        c.op('dve', [kr], [kr], lambda e, r_=r_: e.max(out=r_[:, 72:80], in_=r_[:, 64:72]))
        c.op('dve', [kr], [kr], lambda e, r_=r_: e.tensor_tensor(out=r_[:, 80:81], in0=r_[:, 72:73], in1=r_[:, 73:74], op=ALU.add))
        c.op('dve', [kr], [kr], lambda e, r_=r_: e.reciprocal(out=r_[:, 81:82], in_=r_[:, 80:81]))
        c.op('dve', [kr], [kr], lambda e, r_=r_: e.tensor_tensor(out=r_[:, 82:83], in0=r_[:, 81:82], in1=r_[:, 49:50], op=ALU.mult))
        c.op('dve', [kr], [kr], lambda e, r_=r_: e.tensor_scalar(out=r_[:, 84:92], in0=r_[:, 64:72], scalar1=r_[:, 73:74], scalar2=None, op0=ALU.is_ge))
        c.op('dve', [kr], [kr], lambda e, r_=r_: e.scalar_tensor_tensor(out=r_[:, 84:92], in0=r_[:, 84:92], scalar=r_[:, 82:83], in1=r_[:, 64:72], op0=ALU.mult, op1=ALU.mult))
        for g in range(4):
            c.op('dve', [kr], [kr], lambda e, r_=r_, g=g: e.tensor_scalar(out=r_[:, 96 + 8 * g:104 + 8 * g], in0=r_[:, 84:92], scalar1=r_[:, 36 + g:37 + g], scalar2=None, op0=ALU.mult))
        c.op('pe', [kr, 'identf'], [('ps', 3)], lambda e, r_=r_: e.transpose(c.ps[3][0:32, 0:128], r_[:, 96:128], identf[:]))
        cf = cTf[j % 2]
        cr = cTr[j % 2]
        kcf = ('cTf', j % 2)
        c.op('act', [('ps', 3)], [kcf], lambda e, cf=cf: e.copy(out=cf[:, :], in_=c.ps[3][0:32, 0:128]))
        c.op('dve', [kcf], [('cThi', j)], lambda e, cf=cf, j=j: e.tensor_copy(out=cThi[:, j * 128:(j + 1) * 128], in_=cf[:, :]))
        c.op('dve', [kcf, ('cThi', j)], [('cTr', j % 2)], lambda e, cf=cf, cr=cr, j=j: e.tensor_tensor(out=cr[:, :], in0=cf[:, :], in1=cThi[:, j * 128:(j + 1) * 128], op=ALU.subtract))
        c.op('dve', [('cTr', j % 2)], [('cTlo', j)], lambda e, cr=cr, j=j: e.tensor_copy(out=cTlo[:, j * 128:(j + 1) * 128], in_=cr[:, :]))
    c.barrier()
    c.release(m1)

    NSLOT = 6
    Wg = [c.sb("Wg", [128, 8, 256], BF16) for _ in range(NSLOT)]
    Wu = [c.sb("Wu", [128, 8, 256], BF16) for _ in range(NSLOT)]
    Wd = [c.sb("Wd", [128, 2, 1024], BF16) for _ in range(NSLOT)]
    hid = [c.sb("hid", [128, 2, 512], BF16) for _ in range(4)]
    sil = [c.sb("sil", [128, 512], F32) for _ in range(2)]
    tmp = [c.sb("tmp", [128, 512], F32) for _ in range(2)]

    def load_expert(ex):
        s = ex % NSLOT
        g, e8 = ex // 8, ex % 8
        c.dma('pool', Wg[s][:], A['w_gate'][l, g, e8].rearrange("(c p) f -> p c f", p=128), [], [('Wg', s)], 'wg%d' % s)
        c.dma('pool', Wu[s][:], A['w_up'][l, g, e8].rearrange("(c p) f -> p c f", p=128), [], [('Wu', s)], 'wu%d' % s)
        c.dma('pool', Wd[s][:], A['w_down'][l, g, e8].rearrange("(c p) d -> p c d", p=128), [], [('Wd', s)], 'wd%d' % s)

    for ex in range(NSLOT):
        load_expert(ex)
    nload = NSLOT
    cntm = {'ab': 0, 'y': 0, 'f': 0}
    for G in range(NEXP // 4):
        for bt in range(4):
            for e4 in range(4):
                ex = G * 4 + e4
                s = ex % NSLOT
                hd = hid[e4]
                cb = 6 + ex % 2

                def cbm(e, ex=ex, bt=bt, cb=cb):
                    e.matmul(c.ps[cb][:, :], lhsT=zsel[:, ex * 128:(ex + 1) * 128], rhs=cThi[:, bt * 512:(bt + 1) * 512], start=True, stop=False)
                    return e.matmul(c.ps[cb][:, :], lhsT=zsel[:, ex * 128:(ex + 1) * 128], rhs=cTlo[:, bt * 512:(bt + 1) * 512], start=False, stop=True)
                c.op('pe', ['zsel'] + [('cThi', bt * 4 + q) for q in range(4)] + [('cTlo', bt * 4 + q) for q in range(4)], [('ps', cb)], cbm)
                for fc in range(2):
                    n = cntm['ab']
                    cntm['ab'] += 1
                    ab = (n % 2) * 2
                    bb = ab + 1
                    hk = [('h2T', bt * 4 + q) for q in range(4)]
                    c.op('pe', hk + [('Wg', s)], [('ps', ab)],
                         lambda e, ab=ab, s=s, fc=fc, bt=bt: _acc8(e, c.ps[ab][:, :], lambda cc: Wg[s][:, cc, fc * 128:(fc + 1) * 128], lambda cc: h2T[:, cc, bt * 512:(bt + 1) * 512]))
                    c.op('pe', hk + [('Wu', s)], [('ps', bb)],
                         lambda e, bb=bb, s=s, fc=fc, bt=bt: _acc8(e, c.ps[bb][:, :], lambda cc: Wu[s][:, cc, fc * 128:(fc + 1) * 128], lambda cc: h2T[:, cc, bt * 512:(bt + 1) * 512]))
                    sl = sil[n % 2]
                    tm = tmp[n % 2]
                    c.op('act', [('ps', ab)], [('sil', n % 2)], lambda e, sl=sl, ab=ab: e.activation(out=sl[:], in_=c.ps[ab][:, :], func=AF.Silu))
                    c.op('dve', [('ps', bb), ('sil', n % 2)], [('tmp', n % 2)], lambda e, tm=tm, sl=sl, bb=bb: e.tensor_tensor(out=tm[:], in0=c.ps[bb][:, :], in1=sl[:], op=ALU.mult))
                    c.op('dve', [('tmp', n % 2), ('ps', cb)], [('hid', e4, fc)], lambda e, tm=tm, hd=hd, fc=fc, cb=cb: e.tensor_tensor(out=hd[:, fc, :], in0=tm[:], in1=c.ps[cb][:, :], op=ALU.mult))
            for q in range(4):
                j = bt * 4 + q
                for half in range(2):
                    ny = cntm['y']
                    cntm['y'] += 1
                    yb = 4 + ny % 2

                    def dm(e, yb=yb, q=q, half=half, G=G):
                        inst = None
                        k = 0
                        for e4 in range(4):
                            s = (G * 4 + e4) % NSLOT
                            for fc in range(2):
                                inst = e.matmul(c.ps[yb][:, :], lhsT=hid[e4][:, fc, q * 128:(q + 1) * 128],
                                                rhs=Wd[s][:, fc, half * 512:(half + 1) * 512], start=(k == 0), stop=(k == 7))
                                k += 1
                        return inst
                    c.op('pe', [('hid', e4, fc) for e4 in range(4) for fc in range(2)] + [('Wd', (G * 4 + e4) % NSLOT) for e4 in range(4)],
                         [('ps', yb)], dm)
                    c.op('dve', [('ps', yb), ('xmid', j)], [('xmid', j)],
                         lambda e, yb=yb, j=j, half=half: e.tensor_tensor(out=xmid[:, j, half * 512:(half + 1) * 512],
                                                                         in0=c.ps[yb][:, :], in1=xmid[:, j, half * 512:(half + 1) * 512], op=ALU.add))
        while nload < NEXP and nload < (G + 1) * 4 + NSLOT:
            load_expert(nload)
            nload += 1
    c.barrier()
    c.release(m1)

    if final_norm:
        gfbc = c.sb("gfbc", [128, 1024], F32)
        sq = c.sb("sq3", [128, 1024], BF16)
        st = c.sb("st3", [128, 64], F32)
        ob = [c.sb("ob", [128, 1024], F32) for _ in range(2)]
        c.dma('sp', gfbc[:], A['final_g'].partition_broadcast(128), [], ['gfbc'], 'k25')
        for j in range(NOWN):
            so = j * 4
            kst = ('st', j)
            xj = xmid[:, j, :]
            c.op('act', [('xmid', j)], ['sq', kst],
                 lambda e, xj=xj, so=so: e.activation(out=sq[:], in_=xj, func=AF.Square, accum_out=st[:, so:so + 1]))
            c.op('act', [kst], [kst],
                 lambda e, so=so: e.activation(out=st[:, so + 1:so + 2], in_=st[:, so:so + 1], func=AF.Sqrt, scale=1.0 / D, bias=EPS))
            c.op('dve', [kst], [kst], lambda e, so=so: e.reciprocal(out=st[:, so + 2:so + 3], in_=st[:, so + 1:so + 2]))
            o_ = ob[j % 2]
            c.op('dve', [('xmid', j), kst, 'gfbc'], [('ob', j % 2)],
                 lambda e, o_=o_, xj=xj, so=so: e.scalar_tensor_tensor(out=o_[:], in0=xj, scalar=st[:, so + 2:so + 3], in1=gfbc[:], op0=ALU.mult, op1=ALU.mult))
            c.dma('sp', out_ap[j * 128:(j + 1) * 128, :], o_[:], [('ob', j % 2)], [], 'out%d' % (j % 2))
    else:
        for j in range(NOWN):
            c.dma('sp', out_ap[j * 128:(j + 1) * 128, :], xmid[:, j, :], [('xmid', j)], [], 'out')
    c.barrier()
    c.release(m0)


def _acc8(e, out, lf, rf):
    inst = None
    for cc in range(8):
        inst = e.matmul(out, lhsT=lf(cc), rhs=rf(cc), start=(cc == 0), stop=(cc == 7))
    return inst


W_NAMES = ['norm1_g', 'w_in', 'q_norm_g', 'w_uq', 'kv_norm_g', 'w_ukv', 'pool_w', 'pool_scale', 'w_out',
           'norm2_g', 'w_group', 'b_group', 'w_expert', 'b_expert', 'w_gate', 'w_up', 'w_down', 'final_g']
POOL_WINDOWS = (2, 4, 8, 16)


def host_consts(h):
    cst = {}
    cst['ident'] = np.eye(128, dtype=np.float32)
    j = np.arange(128)
    cst['tri'] = (j[:, None] > j[None, :]).astype(np.float32)
    cst['ones'] = np.ones((128, 128), np.float32)
    cst['trii'] = (j[:, None] >= j[None, :]).astype(np.float32)
    sh = np.zeros((32, 96), np.float32)
    sh[np.arange(32), 64 + np.arange(32)] = 1.0
    cst['shift'] = sh
    sh64 = np.zeros((64, 128), np.float32)
    sh64[np.arange(64), 64 + np.arange(64)] = 1.0
    cst['shift64'] = sh64
    z = np.zeros((32, 4096), np.float32)
    for r in range(32):
        z[r, r * 128:(r + 1) * 128] = 1.0
    cst['zsel'] = z
    k = j[:, None]
    q = j[None, :]
    diag_mla = ((k // 64) <= (q // 64)).astype(np.float32)
    diag_sb = (k < q).astype(np.float32)
    ones = np.ones((128, 128), np.float32)
    zeros = np.zeros((128, 128), np.float32)
    mm = np.zeros((4, 128, 128), np.float32)
    sm = np.zeros((4, 128, 128), np.float32)
    for r in range(2):
        for p in range(2):
            if r == h:
                mm[r * 2 + p] = diag_mla
                sm[r * 2 + p] = diag_sb
            else:
                allowed = g_of(r, p) < g_of(h, p)
                mm[r * 2 + p] = ones if allowed else zeros
                sm[r * 2 + p] = ones if allowed else zeros
    cst['mla_mask'] = mm
    cst['sb_mask'] = sm
    inv = (np.float32(10000.0) ** (-np.arange(0, 32, 2, dtype=np.float32) / np.float32(32))).astype(np.float32)

    def cs_for(blocks):
        pos = (np.asarray(blocks, np.float32)[:, None] * 128 + np.arange(128, dtype=np.float32)[None, :]).reshape(-1)
        ang = (pos[:, None] * inv[None, :]).astype(np.float32)
        return np.cos(ang).astype(np.float32), np.sin(ang).astype(np.float32)
    storage = [g_of(0, i) for i in range(16)] + [g_of(1, i) for i in range(16)]
    cf, sf = cs_for(storage)
    cst['cs_full'] = np.concatenate([cf, sf], axis=1)
    co, so = cs_for([g_of(h, i) for i in range(16)])
    cst['cs_own'] = np.concatenate([co.T, co.T, so.T, so.T], axis=0).astype(np.float32)
    bands = np.zeros((4, 3, 4, 128, 128), np.float32)
    s_ = j[:, None]
    t_ = j[None, :]
    for wi_, w in enumerate(POOL_WINDOWS):
        main = ((s_ <= t_) & (s_ > t_ - w)).astype(np.float32) / w - (s_ == t_).astype(np.float32)
        cntf = np.minimum(t_ + 1, w).astype(np.float32)
        main_first = ((s_ <= t_) & (s_ > t_ - w)).astype(np.float32) / cntf - (s_ == t_).astype(np.float32)
        halo = ((s_ - 128) > (t_ - w)).astype(np.float32) / w
        for var, jj in ((0, 2), (1, 1), (2, 0)):
            gown = g_of(h, jj)
            bands[h, var, wi_] = main_first if gown == 0 else main
            if gown > 0:
                gp = gown - 1
                for which, (r, di) in enumerate([(0, 0), (1, 0), (0, -1), (1, -1)]):
                    ii = jj + di
                    if ii >= 0 and g_of(r, ii) == gp:
                        assert which != h
                        bands[which, var, wi_] = halo
    cst['bands'] = bands.reshape(48, 128, 128)
    return cst


SHARED_CONSTS = {'ident': [128, 128], 'tri': [128, 128], 'trii': [128, 128], 'ones': [128, 128], 'shift': [32, 96], 'shift64': [64, 128],
                 'zsel': [32, 4096], 'cs_full': [4096, 32]}
ROLE_CONSTS = {'mla_mask': [4, 128, 128], 'sb_mask': [4, 128, 128], 'cs_own': [64, 2048], 'bands': [48, 128, 128]}
W_SHAPES = {'norm1_g': [2, 1024], 'w_in': [2, 1024, 1440], 'q_norm_g': [2, 256], 'w_uq': [2, 256, 768],
            'kv_norm_g': [2, 128], 'w_ukv': [2, 128, 1024], 'pool_w': [2, 4, 64, 64], 'pool_scale': [2, 256],
            'w_out': [2, 1024, 1024], 'norm2_g': [2, 1024], 'w_group': [2, 1024, 4], 'b_group': [2, 4],
            'w_expert': [2, 1024, 32], 'b_expert': [2, 32], 'w_gate': [2, 4, 8, 1024, 256],
            'w_up': [2, 4, 8, 1024, 256], 'w_down': [2, 4, 8, 256, 1024], 'final_g': [1024]}


def build_fused_nc(stop_after=None, layers=(0, 1)):
    nc = bass.Bass("TRN2", target_bir_lowering=False)
    A = {}
    for k, shp in W_SHAPES.items():
        A[k] = nc.dram_tensor(k, shp, F32, kind="ExternalInput").ap()
    for k, shp in SHARED_CONSTS.items():
        A[k] = nc.dram_tensor(k, shp, F32, kind="ExternalInput").ap()
    Cs = {}
    for role in ('r0', 'r1', 'own'):
        Cs[role] = {k: nc.dram_tensor("%s_%s" % (k, role), shp, F32, kind="ExternalInput").ap()
                    for k, shp in ROLE_CONSTS.items()}
    sel_d = nc.dram_tensor("sel", [128, 2], F32, kind="ExternalInput").ap()
    x_full = nc.dram_tensor("x_full", [SEQ, D], F32, kind="ExternalInput").ap()
    x1_full = nc.dram_tensor("x1_full", [SEQ, D], F32, kind="Internal").ap()
    out = nc.dram_tensor("out", [SEQ // 2, D], F32, kind="ExternalOutput").ap()
    c = Ctx(nc)
    sel = c.sb("sel", [128, 2], F32)
    xtmp = c.sb("xtmp", [128, 1024], F32)
    c.dma('sp', sel[:], sel_d, [], ['sel'], 'sel')

    def plain_loader(src, r):
        def ld(j, xb, kx, sk):
            c.dma('sp', xb[:], src[(r * 16 + j) * 128:(r * 16 + j + 1) * 128, :], [], [kx], sk)
        return ld

    def select_loader(src):
        def ld(j, xb, kx, sk):
            c.dma('sp', xb[:], src[j * 128:(j + 1) * 128, :], [], [kx], sk)
            c.dma('sp', xtmp[:], src[(16 + j) * 128:(16 + j + 1) * 128, :], [], ['xtmp'], 'xtmp')
            c.op('dve', [kx, 'sel'], [kx],
                 lambda e: e.tensor_scalar(out=xb[:], in0=xb[:], scalar1=sel[:, 0:1], scalar2=None, op0=ALU.mult))
            c.op('dve', [kx, 'xtmp', 'sel'], [kx],
                 lambda e: e.scalar_tensor_tensor(out=xb[:], in0=xtmp[:], scalar=sel[:, 1:2], in1=xb[:],
                                                  op0=ALU.mult, op1=ALU.add))
        return ld
    try:
        if 0 in layers:
            emit_layer(c, 0, A, Cs['r0'], x_full, plain_loader(x_full, 0), x1_full[0:2048, :], False, stop_after)
            emit_layer(c, 0, A, Cs['r1'], x_full, plain_loader(x_full, 1), x1_full[2048:4096, :], False, stop_after)
        if 1 in layers:
            emit_layer(c, 1, A, Cs['own'], x1_full, select_loader(x1_full), out, True, stop_after)
    except StopEmit:
        pass
    return nc


def storage_blocks():
    return [g_of(0, i) for i in range(16)] + [g_of(1, i) for i in range(16)]


def to_storage(xb):
    blk = xb.reshape(NBLK, 128, D)
    return np.ascontiguousarray(blk[storage_blocks()].reshape(SEQ, D))


def make_in_maps(x, weights):
    hc = [host_consts(0), host_consts(1)]
    in_maps = []
    for core in range(8):
        b, h = core // 2, core % 2
        m = dict(weights)
        for k in SHARED_CONSTS:
            m[k] = hc[0][k]
        for k in ROLE_CONSTS:
            m[k + '_r0'] = hc[0][k]
            m[k + '_r1'] = hc[1][k]
            m[k + '_own'] = hc[h][k]
        sel = np.zeros((128, 2), np.float32)
        sel[:, h] = 1.0
        m['sel'] = sel
        m['x_full'] = to_storage(x[b])
        in_maps.append(m)
    return in_maps


def kernel(**inputs):
    x = np.asarray(inputs['x'], np.float32)
    weights = {k: np.ascontiguousarray(np.asarray(inputs[k], np.float32)) for k in W_NAMES}
    nc = build_fused_nc()
    res = run_bass_kernel_spmd(nc, make_in_maps(x, weights), core_ids=list(range(8)))
    y = np.zeros_like(x)
    for core in range(8):
        b, h = core // 2, core % 2
        o = np.asarray(res.results[core]['out']).reshape(NOWN, 128, D)
        yb = y[b].reshape(NBLK, 128, D)
        for i in range(NOWN):
            yb[g_of(h, i)] = o[i]
    return y
```

```python
import numpy as np
import ml_dtypes
import concourse.bass as bass
import concourse.mybir as mybir
from concourse.bass_utils import run_bass_kernel_spmd

F32 = mybir.dt.float32
BF16 = mybir.dt.bfloat16
AF = mybir.ActivationFunctionType
ALU = mybir.AluOpType

D = 1024
SEQ = 4096
NBLK = 32
NOWN = 16
EPS = 1e-6
A_HEADS = 8
B_HEADS = 4
NEXP = 32
SB_BASE = 16512
SB_TOP = 229344


def g_of(r, i):
    return 2 * i + (i % 2) if r == 0 else 2 * i + 1 - (i % 2)


class Ctx:
    def __init__(self, nc):
        self.nc = nc
        self.eng = {'pe': nc.tensor, 'act': nc.scalar, 'dve': nc.vector, 'pool': nc.gpsimd, 'sp': nc.sync}
        self.semh = {}
        self.cnt = {}
        for e in self.eng:
            self.semh['s_' + e] = nc.alloc_semaphore('s_' + e)
            self.cnt['s_' + e] = 0
        self.waited = {e: {} for e in self.eng}
        self.lastw = {}
        self.readers = {}
        self.psacc = {}
        self.sb_off = SB_BASE
        self.limit = SB_TOP
        self.nalloc = 0
        self.ps = [nc.alloc_psum_tensor("psb%d" % i, [128, 512], F32) for i in range(8)]

    def sb(self, name, shape, dtype):
        esz = 2 if dtype == BF16 else 4
        n = 1
        for s in shape[1:]:
            n *= s
        nbytes = (n * esz + 31) // 32 * 32
        off = self.sb_off
        self.sb_off += nbytes
        assert self.sb_off <= self.limit, "SBUF overflow at %s: %d > %d" % (name, self.sb_off, self.limit)
        self.nalloc += 1
        return self.nc.alloc_sbuf_tensor_at("%s_%d" % (name, self.nalloc), list(shape), dtype, offset=off)

    def mark(self):
        return self.sb_off

    def release(self, m):
        self.sb_off = m

    def _deps(self, reads, writes):
        deps = []
        for k in reads:
            t = self.lastw.get(k)
            if t is not None:
                deps.append(t)
        for k in writes:
            t = self.lastw.get(k)
            if t is not None:
                deps.append(t)
            deps.extend(self.readers.get(k, {}).items())
        return deps

    def _emit_waits(self, e, deps, skip_own):
        need = {}
        own = 's_' + e
        for (s, v) in deps:
            if skip_own and s == own:
                continue
            if v > need.get(s, 0):
                need[s] = v
        w = self.waited[e]
        for s, v in need.items():
            if w.get(s, 0) >= v:
                continue
            self.eng[e].wait_ge(self.semh[s], v)
            w[s] = v

    def _commit(self, tok, reads, writes):
        s, v = tok
        for k in reads:
            self.readers.setdefault(k, {})[s] = v
        for k in writes:
            self.lastw[k] = tok
            self.readers[k] = {}

    def op(self, e, reads, writes, fn):
        deps = self._deps(reads, writes)
        s = 's_' + e
        banks = set(k[1] for k in list(reads) + list(writes) if isinstance(k, tuple) and k[0] == 'ps')
        for b in banks:
            for s2, v2 in self.psacc.get(b, {}).items():
                if s2 != s:
                    deps.append((s2, v2))
        self._emit_waits(e, deps, e == 'pe')
        inst = fn(self.eng[e])
        self.cnt[s] += 1
        inst.then_inc(self.semh[s], 1)
        self._commit((s, self.cnt[s]), reads, writes)
        for b in banks:
            self.psacc.setdefault(b, {})[s] = self.cnt[s]

    def dma(self, q, out, in_, reads, writes, semkey, **kw):
        self._emit_waits(q, self._deps(reads, writes), False)
        s = 'd_' + semkey
        if s not in self.semh:
            self.semh[s] = self.nc.alloc_semaphore(s)
            self.cnt[s] = 0
        inst = self.eng[q].dma_start(out=out, in_=in_, **kw)
        self.cnt[s] += 16
        inst.then_inc(self.semh[s], 16)
        self._commit((s, self.cnt[s]), reads, writes)

    def barrier(self):
        for e in self.eng:
            own = 's_' + e
            for s, v in self.cnt.items():
                if s == own or v == 0:
                    continue
                if self.waited[e].get(s, 0) >= v:
                    continue
                self.eng[e].wait_ge(self.semh[s], v)
                self.waited[e][s] = v
        self.lastw = {}
        self.readers = {}
        self.psacc = {}


def psb(c, i):
    return c.ps[i][:].bitcast(BF16)


class StopEmit(Exception):
    pass


def emit_layer(c, l, A, C, x_full, load_own, out_ap, final_norm, stop_after=None):
    nc = c.nc
    m_layer = c.mark()

    def stop_here(tag, dump=None):
        if stop_after != tag:
            return
        c.barrier()
        if dump is not None:
            dump()
        c.barrier()
        raise StopEmit()

    mixedT = c.sb("mixedT", [128, 8, 2048], BF16)
    m_c = c.mark()

    identb = c.sb("identb", [128, 128], BF16)
    identf = c.sb("identf", [128, 128], F32)
    trif = c.sb("trif", [128, 128], F32)
    onesf = c.sb("onesf", [128, 128], F32)
    shiftb = c.sb("shiftb", [32, 96], BF16)
    mmask = c.sb("mmask", [128, 4, 128], F32)
    smask = c.sb("smask", [128, 4, 128], F32)
    g1bc = c.sb("g1bc", [128, 1024], F32)
    gqbc = c.sb("gqbc", [128, 256], F32)
    gkvbc = c.sb("gkvbc", [128, 128], F32)
    Wuq = c.sb("Wuq", [128, 2, 768], BF16)
    Wrot = c.sb("Wrot", [128, 2, 768], BF16)
    Wukv = c.sb("Wukv", [128, 1024], BF16)
    WkPad = c.sb("WkPad", [128, 8, 96], BF16)
    poolw = c.sb("poolw", [64, 4, 64], BF16)
    pscale = c.sb("pscale", [64, 4], F32)

    c.dma('pool', identb[:], A['ident'], [], ['identb'], 'k1')
    c.dma('sp', identf[:], A['ident'], [], ['identf'], 'k2')
    c.dma('sp', trif[:], A['tri'], [], ['trif'], 'k3')
    c.dma('sp', onesf[:], A['ones'], [], ['onesf'], 'k4')
    c.dma('pool', shiftb[:], A['shift'], [], ['shiftb'], 'k5')
    c.dma('sp', mmask[:], C['mla_mask'].rearrange("m k q -> k m q"), [], ['mmask'], 'k6')
    c.dma('sp', smask[:], C['sb_mask'].rearrange("m k q -> k m q"), [], ['smask'], 'k7')
    c.dma('sp', g1bc[:], A['norm1_g'][l].partition_broadcast(128), [], ['g1bc'], 'k8')
    c.dma('sp', gqbc[:], A['q_norm_g'][l].partition_broadcast(128), [], ['gqbc'], 'k9')
    c.dma('sp', gkvbc[:], A['kv_norm_g'][l].partition_broadcast(128), [], ['gkvbc'], 'k10')
    c.dma('pool', Wuq[:], A['w_uq'][l].rearrange("(c p) f -> p c f", p=128), [], ['Wuq'], 'k11')
    c.dma('pool', Wukv[:], A['w_ukv'][l], [], ['Wukv'], 'k12')
    c.dma('pool', poolw[:], A['pool_w'][l].rearrange("g c d -> c g d"), [], ['poolw'], 'k13')
    c.dma('sp', pscale[:], A['pool_scale'][l].rearrange("(g c) -> c g", c=64), [], ['pscale'], 'k14',
          allow_slow_non_contiguous=True)

    c.op('pool', [], ['Wrot'], lambda e: e.memset(Wrot[:], 0.0))
    c.op('pool', [], ['WkPad'], lambda e: e.memset(WkPad[:], 0.0))
    for c2 in range(2):
        src = Wuq[:, c2, :].rearrange("p (h d) -> p h d", d=96)
        dst = Wrot[:, c2, :].rearrange("p (h d) -> p h d", d=96)
        c.op('pool', ['Wuq'], ['Wrot'],
             lambda e, s=src, d_=dst: e.tensor_scalar(out=d_[:, :, 64:80], in0=s[:, :, 80:96], scalar1=-1.0,
                                                      scalar2=None, op0=ALU.mult))
        c.op('pool', ['Wuq'], ['Wrot'],
             lambda e, s=src, d_=dst: e.tensor_copy(out=d_[:, :, 80:96], in_=s[:, :, 64:80]))
    wkv_v = Wukv[:].rearrange("p (h d) -> p h d", d=128)
    c.op('pool', ['Wukv'], ['WkPad'], lambda e: e.tensor_copy(out=WkPad[:, :, 0:64], in_=wkv_v[:, :, 0:64]))

    tmpO = [c.sb("tmpO", [64, 512], BF16) for _ in range(2)]
    cnt_m = {'n': 0}

    def write_mixed(hc16, J, eng, emit_fn, reads):
        ch, odd = hc16 // 2, hc16 % 2
        dst_cols = slice(J * 512, (J + 1) * 512)
        if not odd:
            c.op(eng, reads, [('mixedT', hc16, J)], lambda e: emit_fn(e, mixedT[0:64, ch, dst_cols]))
            return
        n = cnt_m['n']
        cnt_m['n'] += 1
        t_ = tmpO[n % 2]
        kt = ('tmpO', n % 2)
        c.op(eng, reads, [kt], lambda e: emit_fn(e, t_[:, :]))
        c.op('pe', [kt, 'shift64'], [('ps', 7)],
             lambda e: e.matmul(c.ps[7][:, :], lhsT=shift64[:, :], rhs=t_[:, :], start=True, stop=True))
        c.op('act', [('ps', 7)], [('mixedT', hc16, J)],
             lambda e: e.copy(out=mixedT[64:128, ch, dst_cols], in_=c.ps[7][64:128, :]))

    stop_here('A0', lambda: c.dma('sp', out_ap[0:128, :], g1bc[:], ['g1bc'], [], 'out'))
    ckvnT = c.sb("ckvnT", [128, 4096], BF16)
    kropeT = c.sb("kropeT", [32, 4096], BF16)
    cqnT = c.sb("cqnT", [128, 2, 2048], BF16)
    KbT = c.sb("KbT", [128, 2, 4096], BF16)
    QbT = c.sb("QbT", [128, 2, 2048], BF16)
    Vb = c.sb("Vb", [128, 32, 256], BF16)
    ub = c.sb("ub", [128, 32, 256], BF16)
    shift64 = c.sb("shift64", [64, 128], BF16)
    c.dma('pool', shift64[:], A['shift64'], [], ['shift64'], 'shift64')
    m_a = c.mark()

    Win = c.sb("Win", [128, 8, 1440], BF16)
    c.dma('pool', Win[:], A['w_in'][l].rearrange("(c p) f -> p c f", p=128), [], ['Win'], 'k15')
    xbuf = [c.sb("xbuf", [128, 1024], F32) for _ in range(3)]
    hnb = [c.sb("hnb", [128, 1024], BF16) for _ in range(2)]
    sq = c.sb("sq", [128, 1024], BF16)
    hnT = [c.sb("hnT", [128, 8, 512], BF16) for _ in range(2)]
    stat = c.sb("stat", [128, 64], F32)
    ckvn = [c.sb("ckvn", [128, 256], BF16) for _ in range(2)]
    krr = [c.sb("krr", [128, 32], BF16) for _ in range(2)]
    rt = c.sb("rt", [128, 4, 16], F32)
    cst = [c.sb("cst", [128, 4, 32], F32) for _ in range(2)]

    cnt = {'blk': 0, 'bat': 0}

    def front(loader, nb, hT):
        n = cnt['blk']
        cnt['blk'] += 1
        xb = xbuf[n % 3]
        kx = ('x', n % 3)
        loader(xb, kx, 'x%d' % (n % 3))
        so = (n % 8) * 4
        kst = ('st', n % 8)
        c.op('act', [kx], ['sq', kst],
             lambda e: e.activation(out=sq[:], in_=xb[:], func=AF.Square, accum_out=stat[:, so:so + 1]))
        c.op('act', [kst], [kst],
             lambda e: e.activation(out=stat[:, so + 1:so + 2], in_=stat[:, so:so + 1], func=AF.Sqrt,
                                    scale=1.0 / D, bias=EPS))
        c.op('dve', [kst], [kst], lambda e: e.reciprocal(out=stat[:, so + 2:so + 3], in_=stat[:, so + 1:so + 2]))
        hb = hnb[n % 2]
        kh = ('hn', n % 2)
        c.op('dve', [kx, kst, 'g1bc'], [kh],
             lambda e: e.scalar_tensor_tensor(out=hb[:], in0=xb[:], scalar=stat[:, so + 2:so + 3], in1=g1bc[:],
                                              op0=ALU.mult, op1=ALU.mult))
        bank = n % 2
        pv = psb(c, bank)

        def tr(e):
            inst = None
            for cc in range(8):
                inst = e.transpose(pv[:, cc * 128:(cc + 1) * 128], hb[:, cc * 128:(cc + 1) * 128], identb[:])
            return inst
        c.op('pe', [kh, 'identb'], [('ps', bank)], tr)
        ev = 'act' if n % 2 == 0 else 'dve'
        dst = hT[:, :, nb * 128:(nb + 1) * 128]
        srcv = pv[:, :].rearrange("p (c t) -> p c t", t=128)
        if ev == 'act':
            c.op('act', [('ps', bank)], [('hT', id(hT), nb)], lambda e: e.copy(out=dst, in_=srcv))
        else:
            c.op('dve', [('ps', bank)], [('hT', id(hT), nb)], lambda e: e.tensor_copy(out=dst, in_=srcv))

    def mm_acc(bank_ap, lhs_fn, rhs_fn, nk):
        def f(e):
            inst = None
            for cc in range(nk):
                inst = e.matmul(bank_ap, lhsT=lhs_fn(cc), rhs=rhs_fn(cc), start=(cc == 0), stop=(cc == nk - 1))
            return inst
        return f

    def kv_front(b, u):
        hT = hnT[u % 2]
        for nb in range(4):
            slot = b * 4 + nb
            front(lambda xb, kx, sk, slot=slot: c.dma('sp', xb[:], x_full[slot * 128:(slot + 1) * 128, :], [], [kx], sk), nb, hT)

    def kv_proj(b, u):
        hT = hnT[u % 2]
        hkeys = [('hT', id(hT), nb) for nb in range(4)]
        cs_t = cst[b % 2]
        c.dma('sp', cs_t[:], A['cs_full'][b * 512:(b + 1) * 512, :].rearrange("(n p) f -> p n f", p=128),
              [], [('cst', b % 2)], 'cst%d' % (b % 2))
        for nb in range(4):
            slot = b * 4 + nb
            n = cnt['bat']
            cnt['bat'] += 1
            ba = 2 + n % 2
            c.op('pe', [hkeys[nb], 'Win'], [('ps', ba)],
                 mm_acc(c.ps[ba][:, :], lambda cc: hT[:, cc, nb * 128:(nb + 1) * 128],
                        lambda cc: Win[:, cc, 928:1440], 8))
            c.op('act', [('ps', ba)], [('Vb', slot)], lambda e, ba=ba, slot=slot: e.copy(out=Vb[:, slot, :], in_=c.ps[ba][:, 0:256]))
            c.op('dve', [('ps', ba)], [('ub', slot)], lambda e, ba=ba, slot=slot: e.tensor_copy(out=ub[:, slot, :], in_=c.ps[ba][:, 256:512]))
            bb = 4 + n % 2
            c.op('pe', [hkeys[nb], 'Win'], [('ps', bb)],
                 mm_acc(c.ps[bb][:, 0:160], lambda cc: hT[:, cc, nb * 128:(nb + 1) * 128],
                        lambda cc: Win[:, cc, 256:416], 8))
            so = 32 + (n % 8) * 4
            kst = ('st2', n % 8)
            c.op('act', [('ps', bb)], ['sq', kst],
                 lambda e, bb=bb, so=so: e.activation(out=sq[:, 0:128], in_=c.ps[bb][:, 0:128], func=AF.Square,
                                                      accum_out=stat[:, so:so + 1]))
            c.op('act', [kst], [kst],
                 lambda e, so=so: e.activation(out=stat[:, so + 1:so + 2], in_=stat[:, so:so + 1], func=AF.Sqrt,
                                               scale=1.0 / 128, bias=EPS))
            c.op('dve', [kst], [kst],
                 lambda e, so=so: e.reciprocal(out=stat[:, so + 2:so + 3], in_=stat[:, so + 1:so + 2]))
            ck = ckvn[n % 2]
            kck = ('ckvn', n % 2)
            c.op('dve', [('ps', bb), kst, 'gkvbc'], [kck],
                 lambda e, bb=bb, so=so, ck=ck: e.scalar_tensor_tensor(
                     out=ck[:, 0:128], in0=c.ps[bb][:, 0:128], scalar=stat[:, so + 2:so + 3], in1=gkvbc[:],
                     op0=ALU.mult, op1=ALU.mult))
            kr = krr[n % 2]
            kkr = ('krr', n % 2)
            x1 = c.ps[bb][:, 128:144]
            x2 = c.ps[bb][:, 144:160]
            cos = cs_t[:, nb, 0:16]
            sin = cs_t[:, nb, 16:32]
            kcs = ('cst', b % 2)
            c.op('dve', [('ps', bb), kcs], ['rt0'], lambda e, x1=x1, cos=cos: e.tensor_tensor(out=rt[:, 0, :], in0=x1, in1=cos, op=ALU.mult))
            c.op('dve', [('ps', bb), kcs], ['rt1'], lambda e, x2=x2, sin=sin: e.tensor_tensor(out=rt[:, 1, :], in0=x2, in1=sin, op=ALU.mult))
            c.op('dve', [('ps', bb), kcs], ['rt2'], lambda e, x2=x2, cos=cos: e.tensor_tensor(out=rt[:, 2, :], in0=x2, in1=cos, op=ALU.mult))
            c.op('dve', [('ps', bb), kcs], ['rt3'], lambda e, x1=x1, sin=sin: e.tensor_tensor(out=rt[:, 3, :], in0=x1, in1=sin, op=ALU.mult))
            c.op('dve', ['rt0', 'rt1'], [kkr], lambda e, kr=kr: e.tensor_tensor(out=kr[:, 0:16], in0=rt[:, 0, :], in1=rt[:, 1, :], op=ALU.subtract))
            c.op('dve', ['rt2', 'rt3'], [kkr], lambda e, kr=kr: e.tensor_tensor(out=kr[:, 16:32], in0=rt[:, 2, :], in1=rt[:, 3, :], op=ALU.add))
            pv6 = psb(c, 6)

            def tr2(e, ck=ck, kr=kr, nb=nb):
                e.transpose(pv6[:, nb * 128:(nb + 1) * 128], ck[:, 0:128], identb[:])
                return e.transpose(pv6[0:32, 512 + nb * 128:512 + (nb + 1) * 128], kr[:, :], identb[:])
            c.op('pe', [kck, kkr, 'identb'], [('ps', 6)], tr2)
        pv6 = psb(c, 6)
        c.op('dve', [('ps', 6)], [('ckvnT', b)], lambda e, b=b: e.tensor_copy(out=ckvnT[:, b * 512:(b + 1) * 512], in_=pv6[:, 0:512]))
        c.op('dve', [('ps', 6)], [('kropeT', b)], lambda e, b=b: e.tensor_copy(out=kropeT[:, b * 512:(b + 1) * 512], in_=pv6[0:32, 512:1024]))
        for hc in range(2):
            c.op('pe', hkeys + ['Win'], [('ps', 7)],
                 mm_acc(c.ps[7][:, :], lambda cc, hc=hc: Win[:, cc, 672 + hc * 128:672 + (hc + 1) * 128],
                        lambda cc: hT[:, cc, :], 8))
            if hc == 0:
                c.op('act', [('ps', 7)], [('KbT', b, hc)], lambda e, b=b, hc=hc: e.copy(out=KbT[:, hc, b * 512:(b + 1) * 512], in_=c.ps[7][:, :]))
            else:
                c.op('dve', [('ps', 7)], [('KbT', b, hc)], lambda e, b=b, hc=hc: e.tensor_copy(out=KbT[:, hc, b * 512:(b + 1) * 512], in_=c.ps[7][:, :]))

    def own_front(ob, u):
        hT = hnT[u % 2]
        for nb in range(4):
            j = ob * 4 + nb
            front(lambda xb, kx, sk, j=j: load_own(j, xb, kx, sk), nb, hT)

    def own_proj(ob, u):
        hT = hnT[u % 2]
        hkeys = [('hT', id(hT), nb) for nb in range(4)]
        for nb in range(4):
            n = cnt['bat']
            cnt['bat'] += 1
            bb = 4 + n % 2
            c.op('pe', [hkeys[nb], 'Win'], [('ps', bb)],
                 mm_acc(c.ps[bb][:, 0:256], lambda cc: hT[:, cc, nb * 128:(nb + 1) * 128],
                        lambda cc: Win[:, cc, 0:256], 8))
            so = 32 + (n % 8) * 4
            kst = ('st2', n % 8)
            c.op('act', [('ps', bb)], ['sq', kst],
                 lambda e, bb=bb, so=so: e.activation(out=sq[:, 0:256], in_=c.ps[bb][:, 0:256], func=AF.Square,
                                                      accum_out=stat[:, so:so + 1]))
            c.op('act', [kst], [kst],
                 lambda e, so=so: e.activation(out=stat[:, so + 1:so + 2], in_=stat[:, so:so + 1], func=AF.Sqrt,
                                               scale=1.0 / 256, bias=EPS))
            c.op('dve', [kst], [kst],
                 lambda e, so=so: e.reciprocal(out=stat[:, so + 2:so + 3], in_=stat[:, so + 1:so + 2]))
            ck = ckvn[n % 2]
            kck = ('ckvn', n % 2)
            c.op('dve', [('ps', bb), kst, 'gqbc'], [kck],
                 lambda e, bb=bb, so=so, ck=ck: e.scalar_tensor_tensor(
                     out=ck[:, :], in0=c.ps[bb][:, 0:256], scalar=stat[:, so + 2:so + 3], in1=gqbc[:],
                     op0=ALU.mult, op1=ALU.mult))
            pv6 = psb(c, 6)

            def tr3(e, ck=ck, nb=nb):
                e.transpose(pv6[:, nb * 128:(nb + 1) * 128], ck[:, 0:128], identb[:])
                return e.transpose(pv6[:, 512 + nb * 128:512 + (nb + 1) * 128], ck[:, 128:256], identb[:])
            c.op('pe', [kck, 'identb'], [('ps', 6)], tr3)
        pv6 = psb(c, 6)
        c.op('dve', [('ps', 6)], [('cqnT', ob)],
             lambda e, ob=ob: e.tensor_copy(out=cqnT[:, :, ob * 512:(ob + 1) * 512],
                                            in_=pv6[:, :].rearrange("p (c t) -> p c t", t=512)))
        for hc in range(2):
            c.op('pe', hkeys + ['Win'], [('ps', 7)],
                 mm_acc(c.ps[7][:, :], lambda cc, hc=hc: Win[:, cc, 416 + hc * 128:416 + (hc + 1) * 128],
                        lambda cc: hT[:, cc, :], 8))
            if hc == 0:
                c.op('act', [('ps', 7)], [('QbT', ob, hc)], lambda e, ob=ob, hc=hc: e.copy(out=QbT[:, hc, ob * 512:(ob + 1) * 512], in_=c.ps[7][:, :]))
            else:
                c.op('dve', [('ps', 7)], [('QbT', ob, hc)], lambda e, ob=ob, hc=hc: e.tensor_copy(out=QbT[:, hc, ob * 512:(ob + 1) * 512], in_=c.ps[7][:, :]))

    units = [(kv_front, kv_proj, b) for b in range(8)] + [(own_front, own_proj, ob) for ob in range(4)]
    units[0][0](units[0][2], 0)
    for u, (ff, pf, arg) in enumerate(units):
        if u + 1 < len(units):
            units[u + 1][0](units[u + 1][2], u + 1)
        pf(arg, u)

    c.barrier()
    c.release(m_a)
    stop_here('A', None)

    m_p = c.mark()
    bands = c.sb("bands", [128, 48, 128], BF16)
    dT = c.sb("dT", [64, 4, 512], BF16)
    c.dma('pool', bands[:], C['bands'].rearrange("m s t -> s m t"), [], ['bands'], 'k16')
    for ob in range(4):
        for g in range(4):
            def pm(e, ob=ob, g=g):
                inst = None
                for nb in range(4):
                    j = ob * 4 + nb
                    var = 2 if j == 0 else j % 2
                    srcs = [(0, j), (1, j)]
                    if j > 0:
                        srcs += [(0, j - 1), (1, j - 1)]
                    for wi, (r, i) in enumerate(srcs):
                        slot = r * 16 + i
                        inst = e.matmul(c.ps[g][0:64, nb * 128:(nb + 1) * 128],
                                        lhsT=ub[:, slot, g * 64:(g + 1) * 64],
                                        rhs=bands[:, (wi * 3 + var) * 4 + g, :],
                                        start=(wi == 0), stop=(wi == len(srcs) - 1))
                return inst
            c.op('pe', ['bands'], [('ps', g)], pm)
            c.op('act', [('ps', g)], [('dT', g)], lambda e, g=g: e.copy(out=dT[:, g, :], in_=c.ps[g][0:64, :]))
            c.op('pe', [('dT', g), 'poolw'], [('ps', 4 + g % 2)],
                 lambda e, g=g: e.matmul(c.ps[4 + g % 2][0:64, :], lhsT=poolw[:, g, :], rhs=dT[:, g, :], start=True, stop=True))
            write_mixed(12 + g, ob, 'dve',
                        lambda e, o, g=g: e.tensor_scalar(out=o, in0=c.ps[4 + g % 2][0:64, :], scalar1=pscale[:, g:g + 1],
                                                          scalar2=None, op0=ALU.mult),
                        [('ps', 4 + g % 2), 'pscale'])
    c.barrier()
    c.release(m_p)
    stop_here('P', None)

    m_b = c.mark()
    KhT = c.sb("KhT", [128, 4096], BF16)
    Vh = c.sb("Vh", [128, 32, 65], BF16)
    QhT = c.sb("QhT", [128, 2048], BF16)
    csT = [c.sb("csT", [128, 2, 512], F32) for _ in range(2)]
    rq = [c.sb("rq", [128, 2, 512], F32) for _ in range(2)]
    pT = [c.sb("pT", [128, 512], BF16) for _ in range(5)]
    SBK = [0, 1, 4, 5]
    osb = [c.sb("osb", [128, 512], F32) for _ in range(2)]
    rec = c.sb("rec", [128, 512], F32)
    c.op('pool', [], ['Vh1'], lambda e: e.memset(Vh[:, :, 64:65], 1.0))
    SCALE_A = 96.0 ** -0.5
    cnt['q'] = 0
    cnt['s'] = 0
    cnt['o'] = 0
    pend_tail = []

    for h in range(A_HEADS):
        if pend_tail:
            pend_tail.pop()()
        for b in range(8):
            bank = b % 2

            def kb(e, b=b, bank=bank, h=h):
                e.matmul(c.ps[bank][0:96, :], lhsT=WkPad[:, h, :], rhs=ckvnT[:, b * 512:(b + 1) * 512], start=True, stop=False)
                return e.matmul(c.ps[bank][0:96, :], lhsT=shiftb[:, :], rhs=kropeT[:, b * 512:(b + 1) * 512], start=False, stop=True)
            c.op('pe', ['WkPad', 'shiftb', ('ckvnT', b), ('kropeT', b)], [('ps', bank)], kb)
            if b % 2 == 0:
                c.op('act', [('ps', bank)], [('KhT', b)], lambda e, b=b, bank=bank: e.copy(out=KhT[0:96, b * 512:(b + 1) * 512], in_=c.ps[bank][0:96, :]))
            else:
                c.op('dve', [('ps', bank)], [('KhT', b)], lambda e, b=b, bank=bank: e.tensor_copy(out=KhT[0:96, b * 512:(b + 1) * 512], in_=c.ps[bank][0:96, :]))
        for gq in range(4):
            bank = 2 + gq % 2

            def vb(e, gq=gq, bank=bank, h=h):
                inst = None
                for s8 in range(8):
                    slot = gq * 8 + s8
                    inst = e.matmul(c.ps[bank][:, s8 * 64:(s8 + 1) * 64], lhsT=ckvnT[:, slot * 128:(slot + 1) * 128],
                                    rhs=wkv_v[:, h, 64:128], start=True, stop=True)
                return inst
            c.op('pe', ['Wukv'] + [('ckvnT', b) for b in (2 * gq, 2 * gq + 1)], [('ps', bank)], vb)
            ev = 'act' if gq % 2 == 0 else 'dve'
            dst = Vh[:, gq * 8:(gq + 1) * 8, 0:64]
            srcv = c.ps[bank][:, :].rearrange("p (s d) -> p s d", d=64)
            if ev == 'act':
                c.op('act', [('ps', bank)], [('Vh', gq)], lambda e, dst=dst, srcv=srcv: e.copy(out=dst, in_=srcv))
            else:
                c.op('dve', [('ps', bank)], [('Vh', gq)], lambda e, dst=dst, srcv=srcv: e.tensor_copy(out=dst, in_=srcv))
        for ob in range(4):
            nq = cnt['q']
            cnt['q'] += 1
            ct = csT[nq % 2]
            kct = ('csT', nq % 2)
            c.dma('sp', ct[64:96, 0, :], C['cs_own'][0:32, ob * 512:(ob + 1) * 512], [], [kct], 'csT%d' % (nq % 2))
            c.dma('sp', ct[64:96, 1, :], C['cs_own'][32:64, ob * 512:(ob + 1) * 512], [], [kct], 'csT%d' % (nq % 2))

            def qb(e, ob=ob, h=h):
                e.matmul(c.ps[4][0:96, :], lhsT=Wuq[:, 0, h * 96:(h + 1) * 96], rhs=cqnT[:, 0, ob * 512:(ob + 1) * 512], start=True, stop=False)
                e.matmul(c.ps[4][0:96, :], lhsT=Wuq[:, 1, h * 96:(h + 1) * 96], rhs=cqnT[:, 1, ob * 512:(ob + 1) * 512], start=False, stop=True)
                e.matmul(c.ps[5][0:96, :], lhsT=Wrot[:, 0, h * 96:(h + 1) * 96], rhs=cqnT[:, 0, ob * 512:(ob + 1) * 512], start=True, stop=False)
                return e.matmul(c.ps[5][0:96, :], lhsT=Wrot[:, 1, h * 96:(h + 1) * 96], rhs=cqnT[:, 1, ob * 512:(ob + 1) * 512], start=False, stop=True)
            c.op('pe', ['Wuq', 'Wrot', ('cqnT', ob)], [('ps', 4), ('ps', 5)], qb)
            c.op('act', [('ps', 4)], [('QhT', ob)], lambda e, ob=ob: e.copy(out=QhT[0:64, ob * 512:(ob + 1) * 512], in_=c.ps[4][0:64, :]))
            r_ = rq[nq % 2]
            krq = ('rq', nq % 2)
            c.op('dve', [('ps', 4), kct], [krq], lambda e, r_=r_, ct=ct: e.tensor_tensor(out=r_[64:96, 0, :], in0=c.ps[4][64:96, :], in1=ct[64:96, 0, :], op=ALU.mult))
            c.op('dve', [('ps', 5), kct], [krq], lambda e, r_=r_, ct=ct: e.tensor_tensor(out=r_[64:96, 1, :], in0=c.ps[5][64:96, :], in1=ct[64:96, 1, :], op=ALU.mult))
            c.op('dve', [krq], [('QhT', ob)], lambda e, r_=r_, ob=ob: e.tensor_tensor(out=QhT[64:96, ob * 512:(ob + 1) * 512], in0=r_[64:96, 0, :], in1=r_[64:96, 1, :], op=ALU.add))
        for J in range(4):
            j0 = 4 * J
            no = cnt['o']
            cnt['o'] += 1
            obank = 2 + no % 2
            klist = [(r, i) for i in range(j0 + 4) for r in range(2)]
            nk = len(klist)
            base = cnt['s']
            cnt['s'] += nk

            def S_(n, klist=klist, j0=j0, J=J, base=base):
                r, i = klist[n]
                slot = r * 16 + i
                c0 = max(0, i - j0) * 128
                sbank = SBK[(base + n) % 4]
                c.op('pe', [('KhT', slot // 4), ('QhT', J)], [('ps', sbank)],
                     lambda e: e.matmul(c.ps[sbank][:, c0:512], lhsT=KhT[0:96, slot * 128:(slot + 1) * 128],
                                        rhs=QhT[0:96, J * 512 + c0:(J + 1) * 512], start=True, stop=True))

            def E_(n, klist=klist, j0=j0, base=base):
                r, i = klist[n]
                c0 = max(0, i - j0) * 128
                sbank = SBK[(base + n) % 4]
                p_ = pT[(base + n) % 5]
                kp = ('pT', (base + n) % 5)
                c.op('act', [('ps', sbank)], [kp],
                     lambda e: e.activation(out=p_[:, c0:512], in_=c.ps[sbank][:, c0:512], func=AF.Exp, scale=SCALE_A))
                if i >= j0:
                    mi = r * 2 + (i % 2)
                    c.op('pool', [kp, 'mmask'], [kp],
                         lambda e: e.tensor_tensor(out=p_[:, c0:c0 + 128], in0=p_[:, c0:c0 + 128], in1=mmask[:, mi, :], op=ALU.mult))

            def PV_(n, klist=klist, j0=j0, base=base, obank=obank, nk=nk):
                r, i = klist[n]
                slot = r * 16 + i
                c0 = max(0, i - j0) * 128
                p_ = pT[(base + n) % 5]
                kp = ('pT', (base + n) % 5)
                c.op('pe', [kp, ('Vh', slot // 8), 'Vh1'], [('ps', obank)],
                     lambda e: e.matmul(c.ps[obank][0:65, c0:512], lhsT=Vh[:, slot, :], rhs=p_[:, c0:512],
                                        start=(n == 0), stop=(n == nk - 1)))

            def tail(no=no, obank=obank, h=h, J=J):
                o_ = osb[no % 2]
                ko = ('osb', no % 2)
                c.op('act', [('ps', obank)], [ko], lambda e: e.copy(out=o_[0:65, :], in_=c.ps[obank][0:65, :]))
                c.op('pe', [ko, 'onesf'], [('ps', 6)],
                     lambda e: e.matmul(c.ps[6][0:64, :], lhsT=onesf[64:65, 0:64], rhs=o_[64:65, :], start=True, stop=True))
                c.op('dve', [('ps', 6)], ['rec'], lambda e: e.reciprocal(out=rec[0:64, :], in_=c.ps[6][0:64, :]))
                write_mixed(h, J, 'dve',
                            lambda e, o: e.tensor_tensor(out=o, in0=o_[0:64, :], in1=rec[0:64, :], op=ALU.mult),
                            [ko, 'rec'])
            for n in range(min(4, nk)):
                S_(n)
            if pend_tail:
                pend_tail.pop()()
            for n in range(nk):
                E_(n)
                PV_(n)
                if n + 4 < nk:
                    S_(n + 4)
            pend_tail.append(tail)
    if pend_tail:
        pend_tail.pop()()

    c.barrier()
    stop_here('B1', None)
    ebuf = [c.sb("ebuf", [128, 512], F32) for _ in range(2)]
    spb = [c.sb("spb", [128, 512], F32) for _ in range(3)]
    sph = [c.sb("sph", [128, 512], BF16) for _ in range(3)]
    Lacc = c.sb("Lacc", [128, 512], F32)
    Lbf = [c.sb("Lbf", [128, 512], BF16) for _ in range(2)]
    QbTn = c.sb("QbTn", [128, 2048], BF16)
    triib = c.sb("triib", [128, 128], BF16)
    onesb = c.sb("onesb", [128, 128], BF16)
    c.dma('pool', triib[:], A['trii'], [], ['triib'], 'triib')
    c.dma('pool', onesb[:], A['ones'], [], ['onesb'], 'onesb')
    SCALE_B = 64.0 ** -0.5
    cnt['z'] = 0
    for hb in range(B_HEADS):
        pb = (hb % 2) * 64
        hc = hb // 2
        c.op('dve', [], ['QbTn'], lambda e: e.memset(QbTn[:], 0.0))
        c.op('act', [], ['QbTn'],
             lambda e, pb=pb, hc=hc: e.activation(out=QbTn[pb:pb + 64, :], in_=QbT[pb:pb + 64, hc, :], func=AF.Copy,
                                                scale=-SCALE_B))
        for J in range(4):
            j0 = 4 * J
            no = cnt['o']
            cnt['o'] += 1
            obank = 4 + no % 2
            c.op('dve', [], ['Lacc'], lambda e: e.memset(Lacc[:], 0.0))
            klist = []
            for i in range(j0 + 3, -1, -1):
                klist += [(1, i), (0, i)] if i % 2 == 0 else [(0, i), (1, i)]
            nk = len(klist)
            base = cnt['z']
            cnt['z'] += nk

            def info(n, klist=klist, j0=j0, base=base):
                r, i = klist[n]
                g = base + n
                return dict(r=r, i=i, slot=r * 16 + i, c0=max(0, i - j0) * 128, xb=g % 4,
                            e_=ebuf[g % 2], s_=spb[g % 3], sh_=sph[g % 3], a_=pT[g % 5],
                            lb_=Lbf[g % 2], lbn_=Lbf[(g + 1) % 2],
                            ke=('eb', g % 2), ks=('sp', g % 3), ksh=('sph', g % 3), ka=('pT', g % 5),
                            klb=('Lbf', g % 2), klbn=('Lbf', (g + 1) % 2),
                            mi=r * 2 + (i % 2), masked=(i >= j0))

            def stage1(n, info=info, J=J, hc=hc):
                d = info(n)
                c0, xb, slot = d['c0'], d['xb'], d['slot']
                e_, s_, sh_ = d['e_'], d['s_'], d['sh_']
                c.op('pe', [('KbT', slot // 4, hc), 'QbTn'], [('ps', xb)],
                     lambda e: e.matmul(c.ps[xb][:, c0:512], lhsT=KbT[:, hc, slot * 128:(slot + 1) * 128],
                                        rhs=QbTn[:, J * 512 + c0:(J + 1) * 512], start=True, stop=True))
                c.op('act', [('ps', xb)], [d['ke']],
                     lambda e: e.activation(out=e_[:, c0:512], in_=c.ps[xb][:, c0:512], func=AF.Exp, scale=-1.0))
                c.op('act', [d['ke']], [d['ks']],
                     lambda e: e.activation(out=s_[:, c0:512], in_=e_[:, c0:512], func=AF.Ln, bias=1.0, scale=1.0))
                if d['masked']:
                    mi = d['mi']
                    c.op('pool', [d['ks'], 'smask'], [d['ks']],
                         lambda e: e.tensor_tensor(out=s_[:, c0:c0 + 128], in0=s_[:, c0:c0 + 128], in1=smask[:, mi, :], op=ALU.mult))
                c.op('dve', [d['ks']], [d['ksh']], lambda e: e.tensor_copy(out=sh_[:, c0:512], in_=s_[:, c0:512]))

            def stage2a(n, info=info, nk=nk):
                d = info(n)
                c0, xb = d['c0'], d['xb']
                s_, sh_, lb_, lbn_ = d['s_'], d['sh_'], d['lb_'], d['lbn_']

                def cum(e):
                    inst = e.matmul(c.ps[xb][:, c0:512], lhsT=triib[:, :], rhs=sh_[:, c0:512], start=False, stop=(n == 0),
                                    skip_group_check=True)
                    if n > 0:
                        inst = e.matmul(c.ps[xb][:, c0:512], lhsT=onesb[:, :], rhs=lb_[:, c0:512], start=False, stop=True,
                                        skip_group_check=True)
                    return inst
                rd = [d['ksh'], 'triib', 'onesb']
                if n > 0:
                    rd.append(d['klb'])
                c.op('pe', rd, [('ps', xb)], cum)
                if n < nk - 1:
                    c.op('dve', [d['ks'], 'Lacc'], ['Lacc'],
                         lambda e: e.tensor_tensor(out=Lacc[:, c0:512], in0=Lacc[:, c0:512], in1=s_[:, c0:512], op=ALU.add))
                    c.op('dve', ['Lacc'], [d['klbn']], lambda e: e.tensor_copy(out=lbn_[:, :], in_=Lacc[:, :]))

            def stage2b(n, info=info):
                d = info(n)
                c0, xb, a_ = d['c0'], d['xb'], d['a_']
                c.op('act', [('ps', xb)], [d['ka']],
                     lambda e: e.activation(out=a_[:, c0:512], in_=c.ps[xb][:, c0:512], func=AF.Exp, scale=-1.0))
                if d['masked']:
                    mi = d['mi']
                    c.op('pool', [d['ka'], 'smask'], [d['ka']],
                         lambda e: e.tensor_tensor(out=a_[:, c0:c0 + 128], in0=a_[:, c0:c0 + 128], in1=smask[:, mi, :], op=ALU.mult))

            def pv(n, info=info, nk=nk, obank=obank, hb=hb):
                d = info(n)
                c0, slot, a_ = d['c0'], d['slot'], d['a_']
                c.op('pe', [d['ka']], [('ps', obank)],
                     lambda e: e.matmul(c.ps[obank][0:64, c0:512], lhsT=Vb[:, slot, hb * 64:(hb + 1) * 64], rhs=a_[:, c0:512],
                                        start=(n == 0), stop=(n == nk - 1), skip_group_check=True))
            stage1(0)
            stage1(1)
            for n in range(nk):
                stage2a(n)
                if n + 2 < nk:
                    stage1(n + 2)
                stage2b(n)
                if n >= 1:
                    pv(n - 1)
            pv(nk - 1)
            write_mixed(8 + hb, J, 'dve',
                        lambda e, o, obank=obank: e.tensor_copy(out=o, in_=c.ps[obank][0:64, :]),
                        [('ps', obank)])

    c.barrier()
    c.release(m_c)

    XMID_OFF = SB_TOP - 16 * 1024 * 4
    c.nalloc += 1
    xmid = nc.alloc_sbuf_tensor_at("xmid_%d" % c.nalloc, [128, 16, 1024], F32, offset=XMID_OFF)
    c.limit = XMID_OFF
    Wout = c.sb("Wout", [128, 8, 1024], BF16)
    c.dma('pool', Wout[:], A['w_out'][l].rearrange("(hc p) d -> p hc d", p=128), [], ['Wout'], 'Wout')
    xo = [c.sb("xo", [128, 1024], F32) for _ in range(2)]
    for j in range(NOWN):
        xb = xo[j % 2]
        kx = ('xo', j % 2)
        load_own(j, xb, kx, 'xo%d' % (j % 2))
        for half in range(2):
            bank = (j * 2 + half) % 4

            def om(e, bank=bank, j=j, half=half):
                inst = None
                for hc in range(8):
                    inst = e.matmul(c.ps[bank][:, :], lhsT=mixedT[:, hc, j * 128:(j + 1) * 128],
                                    rhs=Wout[:, hc, half * 512:(half + 1) * 512], start=(hc == 0), stop=(hc == 7))
                return inst
            c.op('pe', ['Wout'], [('ps', bank)], om)
            c.op('dve', [('ps', bank), kx], [('xmid', j)],
                 lambda e, bank=bank, j=j, half=half, xb=xb: e.tensor_tensor(
                     out=xmid[:, j, half * 512:(half + 1) * 512], in0=c.ps[bank][:, :], in1=xb[:, half * 512:(half + 1) * 512], op=ALU.add))
    c.barrier()
    c.release(m_layer)

    if stop_after == 'C':
        for j in range(NOWN):
            c.dma('sp', out_ap[j * 128:(j + 1) * 128, :], xmid[:, j, :], [('xmid', j)], [], 'out')
        c.barrier()
        c.limit = SB_TOP
        return

    emit_moe(c, l, A, xmid, out_ap, final_norm)
    c.release(m_layer)
    c.limit = SB_TOP


def emit_moe(c, l, A, xmid, out_ap, final_norm):
    nc = c.nc
    m0 = c.mark()
    h2T = c.sb("h2T", [128, 8, 2048], BF16)
    cThi = c.sb("cThi", [32, 2048], BF16)
    cTlo = c.sb("cTlo", [32, 2048], BF16)
    zsel = c.sb("zsel", [32, 4096], BF16)
    identf = c.sb("identf2", [128, 128], F32)
    c.dma('pool', zsel[:], A['zsel'], [], ['zsel'], max_dma_last_dim=4096, semkey='k18')
    c.dma('sp', identf[:], A['ident'], [], ['identf'], 'k19')
    m1 = c.mark()
    g2bc = c.sb("g2bc", [128, 1024], F32)
    h2f = [c.sb("h2f", [128, 1024], F32) for _ in range(2)]
    h2T32 = [c.sb("h2T32", [128, 8, 128], F32) for _ in range(2)]
    Wr = c.sb("Wr", [128, 8, 36], F32)
    brbc = c.sb("brbc", [128, 36], F32)
    sq = c.sb("sq2", [128, 1024], BF16)
    st = c.sb("st2", [128, 16 * 4], F32)
    lg = c.sb("lg", [128, 16, 36], F32)
    gmax = c.sb("gmax", [128, 16], F32)
    oneh = c.sb("oneh", [128, 16, 4], F32)
    d4 = c.sb("d4", [128, 16, 4], F32)
    eg = c.sb("eg", [128, 16, 4], F32)
    sumg = c.sb("sumg", [128, 16], F32)
    psel = c.sb("psel", [128, 16], F32)
    ein = c.sb("ein", [128, 16, 8], F32)
    tmp8 = c.sb("tmp8", [128, 16, 8], F32)
    emax = c.sb("emax", [128, 16], F32)
    ex = c.sb("ex", [128, 16, 8], F32)
    mk1 = c.sb("mk1", [128, 16, 8], F32)
    v2 = c.sb("v2", [128, 16], F32)
    den = c.sb("den", [128, 16], F32)
    comb = c.sb("comb", [128, 16, 32], F32)
    cTf = c.sb("cTf", [32, 2048], F32)
    cTr = c.sb("cTr", [32, 2048], F32)
    c.dma('sp', g2bc[:], A['norm2_g'][l].partition_broadcast(128), [], ['g2bc'], 'k20')
    c.dma('sp', Wr[:, :, 0:4], A['w_group'][l].rearrange("(c p) f -> p c f", p=128), [], ['Wr_g'], 'k21',
          allow_slow_non_contiguous=True)
    c.dma('sp', Wr[:, :, 4:36], A['w_expert'][l].rearrange("(c p) f -> p c f", p=128), [], ['Wr_e'], 'k22',
          allow_slow_non_contiguous=True)
    c.dma('sp', brbc[:, 0:4], A['b_group'][l].partition_broadcast(128), [], ['brbc_g'], 'k23')
    c.dma('sp', brbc[:, 4:36], A['b_expert'][l].partition_broadcast(128), [], ['brbc_e'], 'k24')

    for j in range(NOWN):
        so = j * 4
        kst = ('st', j)
        xj = xmid[:, j, :]
        c.op('act', [('xmid', j)], ['sq', kst],
             lambda e, xj=xj, so=so: e.activation(out=sq[:], in_=xj, func=AF.Square, accum_out=st[:, so:so + 1]))
        c.op('act', [kst], [kst],
             lambda e, so=so: e.activation(out=st[:, so + 1:so + 2], in_=st[:, so:so + 1], func=AF.Sqrt, scale=1.0 / D, bias=EPS))
        c.op('dve', [kst], [kst], lambda e, so=so: e.reciprocal(out=st[:, so + 2:so + 3], in_=st[:, so + 1:so + 2]))
        hf = h2f[j % 2]
        kh = ('h2f', j % 2)
        c.op('dve', [('xmid', j), kst, 'g2bc'], [kh],
             lambda e, hf=hf, xj=xj, so=so: e.scalar_tensor_tensor(out=hf[:], in0=xj, scalar=st[:, so + 2:so + 3], in1=g2bc[:],
                                                                  op0=ALU.mult, op1=ALU.mult))
        for hf2 in range(2):
            bank = hf2

            def tr(e, hf=hf, hf2=hf2, bank=bank):
                inst = None
                for q in range(4):
                    cc = hf2 * 4 + q
                    inst = e.transpose(c.ps[bank][:, q * 128:(q + 1) * 128], hf[:, cc * 128:(cc + 1) * 128], identf[:])
                return inst
            c.op('pe', [kh, 'identf'], [('ps', bank)], tr)
            t32 = h2T32[j % 2]
            k32 = ('h2T32', j % 2, hf2)
            srcv = c.ps[bank][:, :].rearrange("p (c t) -> p c t", t=128)
            c.op('act', [('ps', bank)], [k32], lambda e, t32=t32, hf2=hf2, srcv=srcv: e.copy(out=t32[:, hf2 * 4:(hf2 + 1) * 4, :], in_=srcv))
            c.op('dve', [('ps', bank)], [('h2T', j)],
                 lambda e, j=j, hf2=hf2, srcv=srcv: e.tensor_copy(out=h2T[:, hf2 * 4:(hf2 + 1) * 4, j * 128:(j + 1) * 128], in_=srcv))
        t32 = h2T32[j % 2]

        def rmm(e, t32=t32):
            inst = None
            for cc in range(8):
                inst = e.matmul(c.ps[2][:, 0:36], lhsT=t32[:, cc, :], rhs=Wr[:, cc, :], start=(cc == 0), stop=(cc == 7))
            return inst
        rbk = 2 + j % 2
        c.op('pe', [('h2T32', j % 2, 0), ('h2T32', j % 2, 1), 'Wr_g', 'Wr_e'], [('ps', rbk)],
             lambda e, t32=t32, rbk=rbk: _acc8(e, c.ps[rbk][:, 0:36], lambda cc: t32[:, cc, :], lambda cc: Wr[:, cc, :]))
        c.op('dve', [('ps', rbk), 'brbc_g', 'brbc_e'], ['lg'],
             lambda e, j=j, rbk=rbk: e.tensor_tensor(out=lg[:, j, :], in0=c.ps[rbk][:, 0:36], in1=brbc[:], op=ALU.add))

    def bc(t2, k):
        return t2[:].unsqueeze(2).broadcast_to([128, 16, k])

    def bcs(t3, g, k):
        return t3[:, :, g:g + 1].broadcast_to([128, 16, k])
    X = mybir.AxisListType.X
    K = 'rt'
    c.op('dve', ['lg'], [K], lambda e: e.tensor_reduce(out=gmax[:], in_=lg[:, :, 0:4], axis=X, op=ALU.max))
    c.op('dve', ['lg', K], [K], lambda e: e.tensor_tensor(out=oneh[:], in0=lg[:, :, 0:4], in1=bc(gmax, 4), op=ALU.is_ge))
    c.op('dve', ['lg', K], [K], lambda e: e.tensor_tensor(out=d4[:], in0=lg[:, :, 0:4], in1=bc(gmax, 4), op=ALU.subtract))
    c.op('act', [K], [K], lambda e: e.activation(out=eg[:], in_=d4[:], func=AF.Exp))
    c.op('dve', [K], [K], lambda e: e.tensor_reduce(out=sumg[:], in_=eg[:], axis=X, op=ALU.add))
    c.op('dve', [K], [K], lambda e: e.reciprocal(out=psel[:], in_=sumg[:]))
    c.op('dve', ['lg', K], [K], lambda e: e.tensor_tensor(out=ein[:], in0=lg[:, :, 4:12], in1=bcs(oneh, 0, 8), op=ALU.mult))
    for g in range(1, 4):
        c.op('dve', ['lg', K], [K], lambda e, g=g: e.tensor_tensor(out=tmp8[:], in0=lg[:, :, 4 + 8 * g:12 + 8 * g], in1=bcs(oneh, g, 8), op=ALU.mult))
        c.op('dve', [K], [K], lambda e: e.tensor_tensor(out=ein[:], in0=ein[:], in1=tmp8[:], op=ALU.add))
    c.op('dve', [K], [K], lambda e: e.tensor_reduce(out=emax[:], in_=ein[:], axis=X, op=ALU.max))
    c.op('dve', [K], [K], lambda e: e.tensor_tensor(out=tmp8[:], in0=ein[:], in1=bc(emax, 8), op=ALU.subtract))
    c.op('act', [K], [K], lambda e: e.activation(out=ex[:], in_=tmp8[:], func=AF.Exp))
    c.op('dve', [K], [K], lambda e: e.tensor_scalar(out=mk1[:], in0=ex[:], scalar1=1.0, scalar2=None, op0=ALU.is_ge))
    c.op('dve', [K], [K], lambda e: e.scalar_tensor_tensor(out=tmp8[:], in0=mk1[:], scalar=-2.0, in1=ex[:], op0=ALU.mult, op1=ALU.add))
    c.op('dve', [K], [K], lambda e: e.tensor_reduce(out=v2[:], in_=tmp8[:], axis=X, op=ALU.max))
    c.op('dve', [K], [K], lambda e: e.tensor_tensor(out=mk1[:], in0=ex[:], in1=bc(v2, 8), op=ALU.is_ge))
    c.op('dve', [K], [K], lambda e: e.tensor_scalar(out=den[:], in0=v2[:], scalar1=1.0, scalar2=None, op0=ALU.add))
    c.op('dve', [K], [K], lambda e: e.reciprocal(out=den[:], in_=den[:]))
    c.op('dve', [K], [K], lambda e: e.tensor_tensor(out=den[:], in0=den[:], in1=psel[:], op=ALU.mult))
    c.op('dve', [K], [K], lambda e: e.tensor_tensor(out=ex[:], in0=ex[:], in1=mk1[:], op=ALU.mult))
    c.op('dve', [K], [K], lambda e: e.tensor_tensor(out=ex[:], in0=ex[:], in1=bc(den, 8), op=ALU.mult))
    for g in range(4):
        c.op('dve', [K], ['comb'], lambda e, g=g: e.tensor_tensor(out=comb[:, :, 8 * g:8 * g + 8], in0=ex[:], in1=bcs(oneh, g, 8), op=ALU.mult))
    for q4 in range(4):
        bank = q4 % 2

        def trc(e, q4=q4, bank=bank):
            inst = None
            for q in range(4):
                inst = e.transpose(c.ps[bank][0:32, q * 128:(q + 1) * 128], comb[:, q4 * 4 + q, :], identf[:])
            return inst
        c.op('pe', ['comb', 'identf'], [('ps', bank)], trc)
        cols = slice(q4 * 512, (q4 + 1) * 512)
        c.op('act', [('ps', bank)], [('cTf', q4)], lambda e, cols=cols, bank=bank: e.copy(out=cTf[:, cols], in_=c.ps[bank][0:32, :]))
        c.op('dve', [('cTf', q4)], [('cThi', q4)], lambda e, cols=cols: e.tensor_copy(out=cThi[:, cols], in_=cTf[:, cols]))
        c.op('dve', [('cTf', q4), ('cThi', q4)], [('cTr', q4)], lambda e, cols=cols: e.tensor_tensor(out=cTr[:, cols], in0=cTf[:, cols], in1=cThi[:, cols], op=ALU.subtract))
        c.op('dve', [('cTr', q4)], [('cTlo', q4)], lambda e, cols=cols: e.tensor_copy(out=cTlo[:, cols], in_=cTr[:, cols]))
    c.barrier()
    c.release(m1)

    NSLOT = 6
    Wg = [c.sb("Wg", [128, 8, 256], BF16) for _ in range(NSLOT)]
    Wu = [c.sb("Wu", [128, 8, 256], BF16) for _ in range(NSLOT)]
    Wd = [c.sb("Wd", [128, 2, 1024], BF16) for _ in range(NSLOT)]
    hid = [c.sb("hid", [128, 2, 512], BF16) for _ in range(4)]
    sil = [c.sb("sil", [128, 512], F32) for _ in range(2)]
    tmp = [c.sb("tmp", [128, 512], F32) for _ in range(2)]

    def load_expert(ex):
        s = ex % NSLOT
        g, e8 = ex // 8, ex % 8
        c.dma('pool', Wg[s][:], A['w_gate'][l, g, e8].rearrange("(c p) f -> p c f", p=128), [], [('Wg', s)], 'wg%d' % s)
        c.dma('pool', Wu[s][:], A['w_up'][l, g, e8].rearrange("(c p) f -> p c f", p=128), [], [('Wu', s)], 'wu%d' % s)
        c.dma('pool', Wd[s][:], A['w_down'][l, g, e8].rearrange("(c p) d -> p c d", p=128), [], [('Wd', s)], 'wd%d' % s)

    for ex in range(NSLOT):
        load_expert(ex)
    nload = NSLOT
    cntm = {'ab': 0, 'y': 0, 'f': 0}
    for G in range(NEXP // 4):
        for bt in range(4):
            for e4 in range(4):
                ex = G * 4 + e4
                s = ex % NSLOT
                hd = hid[e4]
                cb = 6 + ex % 2

                def cbm(e, ex=ex, bt=bt, cb=cb):
                    e.matmul(c.ps[cb][:, :], lhsT=zsel[:, ex * 128:(ex + 1) * 128], rhs=cThi[:, bt * 512:(bt + 1) * 512], start=True, stop=False)
                    return e.matmul(c.ps[cb][:, :], lhsT=zsel[:, ex * 128:(ex + 1) * 128], rhs=cTlo[:, bt * 512:(bt + 1) * 512], start=False, stop=True)
                c.op('pe', ['zsel'] + [('cThi', bt * 4 + q) for q in range(4)] + [('cTlo', bt * 4 + q) for q in range(4)], [('ps', cb)], cbm)
                for fc in range(2):
                    n = cntm['ab']
                    cntm['ab'] += 1
                    ab = (n % 2) * 2
                    bb = ab + 1
                    hk = [('h2T', bt * 4 + q) for q in range(4)]
                    c.op('pe', hk + [('Wg', s)], [('ps', ab)],
                         lambda e, ab=ab, s=s, fc=fc, bt=bt: _acc8(e, c.ps[ab][:, :], lambda cc: Wg[s][:, cc, fc * 128:(fc + 1) * 128], lambda cc: h2T[:, cc, bt * 512:(bt + 1) * 512]))
                    c.op('pe', hk + [('Wu', s)], [('ps', bb)],
                         lambda e, bb=bb, s=s, fc=fc, bt=bt: _acc8(e, c.ps[bb][:, :], lambda cc: Wu[s][:, cc, fc * 128:(fc + 1) * 128], lambda cc: h2T[:, cc, bt * 512:(bt + 1) * 512]))
                    sl = sil[n % 2]
                    tm = tmp[n % 2]
                    c.op('act', [('ps', ab)], [('sil', n % 2)], lambda e, sl=sl, ab=ab: e.activation(out=sl[:], in_=c.ps[ab][:, :], func=AF.Silu))
                    c.op('dve', [('ps', bb), ('sil', n % 2)], [('tmp', n % 2)], lambda e, tm=tm, sl=sl, bb=bb: e.tensor_tensor(out=tm[:], in0=c.ps[bb][:, :], in1=sl[:], op=ALU.mult))
                    c.op('dve', [('tmp', n % 2), ('ps', cb)], [('hid', e4, fc)], lambda e, tm=tm, hd=hd, fc=fc, cb=cb: e.tensor_tensor(out=hd[:, fc, :], in0=tm[:], in1=c.ps[cb][:, :], op=ALU.mult))
            for q in range(4):
                j = bt * 4 + q
                for half in range(2):
                    ny = cntm['y']
                    cntm['y'] += 1
                    yb = 4 + ny % 2

                    def dm(e, yb=yb, q=q, half=half, G=G):
                        inst = None
                        k = 0
                        for e4 in range(4):
                            s = (G * 4 + e4) % NSLOT
                            for fc in range(2):
                                inst = e.matmul(c.ps[yb][:, :], lhsT=hid[e4][:, fc, q * 128:(q + 1) * 128],
                                                rhs=Wd[s][:, fc, half * 512:(half + 1) * 512], start=(k == 0), stop=(k == 7))
                                k += 1
                        return inst
                    c.op('pe', [('hid', e4, fc) for e4 in range(4) for fc in range(2)] + [('Wd', (G * 4 + e4) % NSLOT) for e4 in range(4)],
                         [('ps', yb)], dm)
                    c.op('dve', [('ps', yb), ('xmid', j)], [('xmid', j)],
                         lambda e, yb=yb, j=j, half=half: e.tensor_tensor(out=xmid[:, j, half * 512:(half + 1) * 512],
                                                                         in0=c.ps[yb][:, :], in1=xmid[:, j, half * 512:(half + 1) * 512], op=ALU.add))
        while nload < NEXP and nload < (G + 1) * 4 + NSLOT:
            load_expert(nload)
            nload += 1
    c.barrier()
    c.release(m1)

    if final_norm:
        gfbc = c.sb("gfbc", [128, 1024], F32)
        sq = c.sb("sq3", [128, 1024], BF16)
        st = c.sb("st3", [128, 64], F32)
        ob = [c.sb("ob", [128, 1024], F32) for _ in range(2)]
        c.dma('sp', gfbc[:], A['final_g'].partition_broadcast(128), [], ['gfbc'], 'k25')
        for j in range(NOWN):
            so = j * 4
            kst = ('st', j)
            xj = xmid[:, j, :]
            c.op('act', [('xmid', j)], ['sq', kst],
                 lambda e, xj=xj, so=so: e.activation(out=sq[:], in_=xj, func=AF.Square, accum_out=st[:, so:so + 1]))
            c.op('act', [kst], [kst],
                 lambda e, so=so: e.activation(out=st[:, so + 1:so + 2], in_=st[:, so:so + 1], func=AF.Sqrt, scale=1.0 / D, bias=EPS))
            c.op('dve', [kst], [kst], lambda e, so=so: e.reciprocal(out=st[:, so + 2:so + 3], in_=st[:, so + 1:so + 2]))
            o_ = ob[j % 2]
            c.op('dve', [('xmid', j), kst, 'gfbc'], [('ob', j % 2)],
                 lambda e, o_=o_, xj=xj, so=so: e.scalar_tensor_tensor(out=o_[:], in0=xj, scalar=st[:, so + 2:so + 3], in1=gfbc[:], op0=ALU.mult, op1=ALU.mult))
            c.dma('sp', out_ap[j * 128:(j + 1) * 128, :], o_[:], [('ob', j % 2)], [], 'out%d' % (j % 2))
    else:
        for j in range(NOWN):
            c.dma('sp', out_ap[j * 128:(j + 1) * 128, :], xmid[:, j, :], [('xmid', j)], [], 'out')
    c.barrier()
    c.release(m0)


def _acc8(e, out, lf, rf):
    inst = None
    for cc in range(8):
        inst = e.matmul(out, lhsT=lf(cc), rhs=rf(cc), start=(cc == 0), stop=(cc == 7))
    return inst


W_NAMES = ['norm1_g', 'w_in', 'q_norm_g', 'w_uq', 'kv_norm_g', 'w_ukv', 'pool_w', 'pool_scale', 'w_out',
           'norm2_g', 'w_group', 'b_group', 'w_expert', 'b_expert', 'w_gate', 'w_up', 'w_down', 'final_g']
POOL_WINDOWS = (2, 4, 8, 16)


def host_consts(h):
    cst = {}
    cst['ident'] = np.eye(128, dtype=np.float32)
    j = np.arange(128)
    cst['tri'] = (j[:, None] > j[None, :]).astype(np.float32)
    cst['ones'] = np.ones((128, 128), np.float32)
    cst['trii'] = (j[:, None] >= j[None, :]).astype(np.float32)
    sh = np.zeros((32, 96), np.float32)
    sh[np.arange(32), 64 + np.arange(32)] = 1.0
    cst['shift'] = sh
    sh64 = np.zeros((64, 128), np.float32)
    sh64[np.arange(64), 64 + np.arange(64)] = 1.0
    cst['shift64'] = sh64
    z = np.zeros((32, 4096), np.float32)
    for r in range(32):
        z[r, r * 128:(r + 1) * 128] = 1.0
    cst['zsel'] = z
    k = j[:, None]
    q = j[None, :]
    diag_mla = ((k // 64) <= (q // 64)).astype(np.float32)
    diag_sb = (k < q).astype(np.float32)
    ones = np.ones((128, 128), np.float32)
    zeros = np.zeros((128, 128), np.float32)
    mm = np.zeros((4, 128, 128), np.float32)
    sm = np.zeros((4, 128, 128), np.float32)
    for r in range(2):
        for p in range(2):
            if r == h:
                mm[r * 2 + p] = diag_mla
                sm[r * 2 + p] = diag_sb
            else:
                allowed = g_of(r, p) < g_of(h, p)
                mm[r * 2 + p] = ones if allowed else zeros
                sm[r * 2 + p] = ones if allowed else zeros
    cst['mla_mask'] = mm
    cst['sb_mask'] = sm
    inv = (np.float32(10000.0) ** (-np.arange(0, 32, 2, dtype=np.float32) / np.float32(32))).astype(np.float32)

    def cs_for(blocks):
        pos = (np.asarray(blocks, np.float32)[:, None] * 128 + np.arange(128, dtype=np.float32)[None, :]).reshape(-1)
        ang = (pos[:, None] * inv[None, :]).astype(np.float32)
        return np.cos(ang).astype(np.float32), np.sin(ang).astype(np.float32)
    storage = [g_of(0, i) for i in range(16)] + [g_of(1, i) for i in range(16)]
    cf, sf = cs_for(storage)
    cst['cs_full'] = np.concatenate([cf, sf], axis=1)
    co, so = cs_for([g_of(h, i) for i in range(16)])
    cst['cs_own'] = np.concatenate([co.T, co.T, so.T, so.T], axis=0).astype(np.float32)
    bands = np.zeros((4, 3, 4, 128, 128), np.float32)
    s_ = j[:, None]
    t_ = j[None, :]
    for wi_, w in enumerate(POOL_WINDOWS):
        main = ((s_ <= t_) & (s_ > t_ - w)).astype(np.float32) / w - (s_ == t_).astype(np.float32)
        cntf = np.minimum(t_ + 1, w).astype(np.float32)
        main_first = ((s_ <= t_) & (s_ > t_ - w)).astype(np.float32) / cntf - (s_ == t_).astype(np.float32)
        halo = ((s_ - 128) > (t_ - w)).astype(np.float32) / w
        for var, jj in ((0, 2), (1, 1), (2, 0)):
            gown = g_of(h, jj)
            bands[h, var, wi_] = main_first if gown == 0 else main
            if gown > 0:
                gp = gown - 1
                for which, (r, di) in enumerate([(0, 0), (1, 0), (0, -1), (1, -1)]):
                    ii = jj + di
                    if ii >= 0 and g_of(r, ii) == gp:
                        assert which != h
                        bands[which, var, wi_] = halo
    cst['bands'] = bands.reshape(48, 128, 128)
    return cst


SHARED_CONSTS = {'ident': [128, 128], 'tri': [128, 128], 'trii': [128, 128], 'ones': [128, 128], 'shift': [32, 96], 'shift64': [64, 128],
                 'zsel': [32, 4096], 'cs_full': [4096, 32]}
ROLE_CONSTS = {'mla_mask': [4, 128, 128], 'sb_mask': [4, 128, 128], 'cs_own': [64, 2048], 'bands': [48, 128, 128]}
W_SHAPES = {'norm1_g': [2, 1024], 'w_in': [2, 1024, 1440], 'q_norm_g': [2, 256], 'w_uq': [2, 256, 768],
            'kv_norm_g': [2, 128], 'w_ukv': [2, 128, 1024], 'pool_w': [2, 4, 64, 64], 'pool_scale': [2, 256],
            'w_out': [2, 1024, 1024], 'norm2_g': [2, 1024], 'w_group': [2, 1024, 4], 'b_group': [2, 4],
            'w_expert': [2, 1024, 32], 'b_expert': [2, 32], 'w_gate': [2, 4, 8, 1024, 256],
            'w_up': [2, 4, 8, 1024, 256], 'w_down': [2, 4, 8, 256, 1024], 'final_g': [1024]}


def build_fused_nc(stop_after=None, layers=(0, 1)):
    nc = bass.Bass("TRN2", target_bir_lowering=False)
    A = {}
    for k, shp in W_SHAPES.items():
        A[k] = nc.dram_tensor(k, shp, F32, kind="ExternalInput").ap()
    for k, shp in SHARED_CONSTS.items():
        A[k] = nc.dram_tensor(k, shp, F32, kind="ExternalInput").ap()
    Cs = {}
    for role in ('r0', 'r1', 'own'):
        Cs[role] = {k: nc.dram_tensor("%s_%s" % (k, role), shp, F32, kind="ExternalInput").ap()
                    for k, shp in ROLE_CONSTS.items()}
    sel_d = nc.dram_tensor("sel", [128, 2], F32, kind="ExternalInput").ap()
    x_full = nc.dram_tensor("x_full", [SEQ, D], F32, kind="ExternalInput").ap()
    x1_full = nc.dram_tensor("x1_full", [SEQ, D], F32, kind="Internal").ap()
    out = nc.dram_tensor("out", [SEQ // 2, D], F32, kind="ExternalOutput").ap()
    c = Ctx(nc)
    sel = c.sb("sel", [128, 2], F32)
    xtmp = c.sb("xtmp", [128, 1024], F32)
    c.dma('sp', sel[:], sel_d, [], ['sel'], 'sel')

    def plain_loader(src, r):
        def ld(j, xb, kx, sk):
            c.dma('sp', xb[:], src[(r * 16 + j) * 128:(r * 16 + j + 1) * 128, :], [], [kx], sk)
        return ld

    def select_loader(src):
        def ld(j, xb, kx, sk):
            c.dma('sp', xb[:], src[j * 128:(j + 1) * 128, :], [], [kx], sk)
            c.dma('sp', xtmp[:], src[(16 + j) * 128:(16 + j + 1) * 128, :], [], ['xtmp'], 'xtmp')
            c.op('dve', [kx, 'sel'], [kx],
                 lambda e: e.tensor_scalar(out=xb[:], in0=xb[:], scalar1=sel[:, 0:1], scalar2=None, op0=ALU.mult))
            c.op('dve', [kx, 'xtmp', 'sel'], [kx],
                 lambda e: e.scalar_tensor_tensor(out=xb[:], in0=xtmp[:], scalar=sel[:, 1:2], in1=xb[:],
                                                  op0=ALU.mult, op1=ALU.add))
        return ld
    try:
        if 0 in layers:
            emit_layer(c, 0, A, Cs['r0'], x_full, plain_loader(x_full, 0), x1_full[0:2048, :], False, stop_after)
            emit_layer(c, 0, A, Cs['r1'], x_full, plain_loader(x_full, 1), x1_full[2048:4096, :], False, stop_after)
        if 1 in layers:
            emit_layer(c, 1, A, Cs['own'], x1_full, select_loader(x1_full), out, True, stop_after)
    except StopEmit:
        pass
    return nc


def storage_blocks():
    return [g_of(0, i) for i in range(16)] + [g_of(1, i) for i in range(16)]


def to_storage(xb):
    blk = xb.reshape(NBLK, 128, D)
    return np.ascontiguousarray(blk[storage_blocks()].reshape(SEQ, D))


def make_in_maps(x, weights):
    hc = [host_consts(0), host_consts(1)]
    in_maps = []
    for core in range(8):
        b, h = core // 2, core % 2
        m = dict(weights)
        for k in SHARED_CONSTS:
            m[k] = hc[0][k]
        for k in ROLE_CONSTS:
            m[k + '_r0'] = hc[0][k]
            m[k + '_r1'] = hc[1][k]
            m[k + '_own'] = hc[h][k]
        sel = np.zeros((128, 2), np.float32)
        sel[:, h] = 1.0
        m['sel'] = sel
        m['x_full'] = to_storage(x[b])
        in_maps.append(m)
    return in_maps


def kernel(**inputs):
    x = np.asarray(inputs['x'], np.float32)
    weights = {k: np.ascontiguousarray(np.asarray(inputs[k], np.float32)) for k in W_NAMES}
    nc = build_fused_nc()
    res = run_bass_kernel_spmd(nc, make_in_maps(x, weights), core_ids=list(range(8)))
    y = np.zeros_like(x)
    for core in range(8):
        b, h = core // 2, core % 2
        o = np.asarray(res.results[core]['out']).reshape(NOWN, 128, D)
        yb = y[b].reshape(NBLK, 128, D)
        for i in range(NOWN):
            yb[g_of(h, i)] = o[i]
    return y
```
